# Optimizing a Trainium2 kernel written in Bass

```python
import jax, jax.numpy as jnp
from jax import lax
import numpy as np

D_MODEL = 1024
BATCH = 2
SEQ = 16384
DEPTH = 2

HEAD_DIM = 64
ROT_DIM = HEAD_DIM // 4
ROPE_THETA = 500000.0
NSA_HEADS = 8
NSA_KV_HEADS = 2
NSA_GROUP = NSA_HEADS // NSA_KV_HEADS
CMP_LEN = 32
CMP_STRIDE = 16
CMP_HID = 2 * HEAD_DIM
SLC_LEN = 64
SLC_TOPN = 16
WIN = 512
MOBA_HEADS = 8
MOBA_BLOCK = 256
MOBA_TOPK = 3
Q_CHUNK = 64
PEER_HEADS = 8
PEER_NKEYS = 128
PEER_EXPERTS = PEER_NKEYS * PEER_NKEYS
PEER_QDIM = 256
PEER_TOPK = 16
PEER_CHUNK = 128
RMS_EPS = 1e-6
NEG = -1e30
SEL_FORCE = 1e4

NSA_Q = NSA_HEADS * HEAD_DIM
NSA_KV = NSA_KV_HEADS * HEAD_DIM
MOBA_W = MOBA_HEADS * HEAD_DIM
IN_COLS = NSA_Q + 6 * NSA_KV + 3 * NSA_HEADS + 3 * MOBA_W + 2 * D_MODEL

kernel_name = "hybrid_nsa_moba_peer_adaln"


def rms_norm(x, g):
    xf = x.astype(jnp.float32)
    y = xf * lax.rsqrt(jnp.mean(xf * xf, axis=-1, keepdims=True) + RMS_EPS)
    return (y * g.astype(jnp.float32)).astype(x.dtype)


def partial_rope(x, pos):
    half = ROT_DIM // 2
    inv = ROPE_THETA ** (-jnp.arange(half, dtype=jnp.float32) / half)
    ang = pos.astype(jnp.float32)[:, None] * inv[None, :]
    cos = jnp.cos(ang)[None, :, None, :].astype(x.dtype)
    sin = jnp.sin(ang)[None, :, None, :].astype(x.dtype)
    x1, x2, rest = x[..., :half], x[..., half:ROT_DIM], x[..., ROT_DIM:]
    return jnp.concatenate([x1 * cos - x2 * sin, x2 * cos + x1 * sin, rest], axis=-1)


def masked_softmax(s, mask):
    s = jnp.where(mask, s.astype(jnp.float32), NEG)
    m = jnp.max(s, axis=-1, keepdims=True)
    p = jnp.where(mask, jnp.exp(s - m), 0.0)
    return p / jnp.maximum(jnp.sum(p, axis=-1, keepdims=True), 1e-30)


def split_cols(x, sizes):
    out, o = [], 0
    for n in sizes:
        out.append(x[..., o:o + n])
        o += n
    return out


def nsa_compress(k, pe, w1, w2):
    b, s = k.shape[:2]
    nc = (s - CMP_LEN) // CMP_STRIDE + 1
    idx = jnp.arange(nc)[:, None] * CMP_STRIDE + jnp.arange(CMP_LEN)[None, :]
    blk = k[:, idx] + pe[None, None, :, None, :]
    blk = jnp.transpose(blk, (0, 1, 3, 2, 4)).reshape(b, nc, NSA_KV_HEADS, CMP_LEN * HEAD_DIM)
    return jax.nn.gelu(blk @ w1) @ w2


def nsa_attention(q_rot, q_raw, kc, vc, ks, vs, kw, vw, gates):
    b, s = q_rot.shape[:2]
    nc = kc.shape[1]
    ns = s // SLC_LEN
    n_sel = min(SLC_TOPN, ns)
    scale = HEAD_DIM ** -0.5
    c_start = jnp.arange(nc) * CMP_STRIDE
    cmp_end = c_start + CMP_LEN - 1
    s_start = jnp.arange(ns) * SLC_LEN
    overlap = jnp.maximum(
        jnp.minimum(c_start[:, None] + CMP_LEN, s_start[None, :] + SLC_LEN)
        - jnp.maximum(c_start[:, None], s_start[None, :]), 0).astype(jnp.float32) / CMP_LEN
    ks_blk = ks.reshape(b, ns, SLC_LEN, NSA_KV_HEADS, HEAD_DIM).transpose(0, 3, 1, 2, 4)
    vs_blk = vs.reshape(b, ns, SLC_LEN, NSA_KV_HEADS, HEAD_DIM).transpose(0, 3, 1, 2, 4)
    kw_pad = jnp.pad(kw, ((0, 0), (WIN, 0), (0, 0), (0, 0)))
    vw_pad = jnp.pad(vw, ((0, 0), (WIN, 0), (0, 0), (0, 0)))
    bi = jnp.arange(b)[:, None, None, None]
    hi = jnp.arange(NSA_KV_HEADS)[None, :, None, None]
    blk_id = jnp.arange(ns)

    def chunk(i):
        s0 = i * Q_CHUNK
        t = s0 + jnp.arange(Q_CHUNK)
        qr = lax.dynamic_slice_in_dim(q_rot, s0, Q_CHUNK, 1).reshape(b, Q_CHUNK, NSA_KV_HEADS, NSA_GROUP, HEAD_DIM)
        qu = lax.dynamic_slice_in_dim(q_raw, s0, Q_CHUNK, 1).reshape(b, Q_CHUNK, NSA_KV_HEADS, NSA_GROUP, HEAD_DIM)
        g = lax.dynamic_slice_in_dim(gates, s0, Q_CHUNK, 1)
        sc = jnp.einsum('bqkgd,bckd->bkgqc', qu, kc).astype(jnp.float32) * scale
        p_c = masked_softmax(sc, cmp_end[None, :] <= t[:, None])
        o_c = jnp.einsum('bkgqc,bckd->bqkgd', p_c.astype(vc.dtype), vc)
        imp = jnp.einsum('bkgqc,cn->bkqn', p_c, overlap)
        cur = t // SLC_LEN
        valid = blk_id[None, :] <= cur[:, None]
        forced = (blk_id[None, :] == 0) | (blk_id[None, :] == cur[:, None]) | (blk_id[None, :] == cur[:, None] - 1)
        score = jnp.where(valid, jnp.where(forced, SEL_FORCE, imp), NEG)
        vals, idx = lax.top_k(score, n_sel)
        ok = vals > NEG * 0.5
        kg = ks_blk[bi, hi, idx]
        vg = vs_blk[bi, hi, idx]
        kpos = idx[..., None] * SLC_LEN + jnp.arange(SLC_LEN)
        smask = (ok[..., None] & (kpos <= t[None, None, :, None, None])).reshape(b, NSA_KV_HEADS, 1, Q_CHUNK, n_sel * SLC_LEN)
        ss = jnp.einsum('bqkgd,bkqnld->bkgqnl', qr, kg).reshape(b, NSA_KV_HEADS, NSA_GROUP, Q_CHUNK, n_sel * SLC_LEN)
        p_s = masked_softmax(ss.astype(jnp.float32) * scale, smask)
        p_s = p_s.reshape(b, NSA_KV_HEADS, NSA_GROUP, Q_CHUNK, n_sel, SLC_LEN).astype(vg.dtype)
        o_s = jnp.einsum('bkgqnl,bkqnld->bqkgd', p_s, vg)
        kwc = lax.dynamic_slice_in_dim(kw_pad, s0, WIN + Q_CHUNK, 1)
        vwc = lax.dynamic_slice_in_dim(vw_pad, s0, WIN + Q_CHUNK, 1)
        wpos = s0 - WIN + jnp.arange(WIN + Q_CHUNK)
        wmask = (wpos[None, :] <= t[:, None]) & (wpos[None, :] > t[:, None] - WIN) & (wpos[None, :] >= 0)
        sw = jnp.einsum('bqkgd,bjkd->bkgqj', qr, kwc).astype(jnp.float32) * scale
        p_w = masked_softmax(sw, wmask)
        o_w = jnp.einsum('bkgqj,bjkd->bqkgd', p_w.astype(vwc.dtype), vwc)
        shp = (b, Q_CHUNK, NSA_HEADS, HEAD_DIM)
        return (g[..., 0:1] * o_c.reshape(shp) + g[..., 1:2] * o_s.reshape(shp) + g[..., 2:3] * o_w.reshape(shp))

    out = lax.map(chunk, jnp.arange(s // Q_CHUNK))
    return jnp.transpose(out, (1, 0, 2, 3, 4)).reshape(b, s, NSA_Q)


def moba_attention(q, k, v):
    b, s = q.shape[:2]
    nb = -(-s // MOBA_BLOCK)
    pad = nb * MOBA_BLOCK - s
    n_top = min(MOBA_TOPK, nb)
    scale = HEAD_DIM ** -0.5
    k_pad = jnp.pad(k, ((0, 0), (0, pad), (0, 0), (0, 0)))
    v_pad = jnp.pad(v, ((0, 0), (0, pad), (0, 0), (0, 0)))
    k_blk = k_pad.reshape(b, nb, MOBA_BLOCK, MOBA_HEADS, HEAD_DIM).transpose(0, 3, 1, 2, 4)
    v_blk = v_pad.reshape(b, nb, MOBA_BLOCK, MOBA_HEADS, HEAD_DIM).transpose(0, 3, 1, 2, 4)
    k_mean = jnp.mean(k_blk.astype(jnp.float32), axis=3)
    bi = jnp.arange(b)[:, None, None, None]
    hi = jnp.arange(MOBA_HEADS)[None, :, None, None]

    def chunk(i):
        s0 = i * Q_CHUNK
        t = s0 + jnp.arange(Q_CHUNK)
        cur = s0 // MOBA_BLOCK
        qc = lax.dynamic_slice_in_dim(q, s0, Q_CHUNK, 1)
        gs = jnp.einsum('bqhd,bhnd->bhqn', qc.astype(jnp.float32), k_mean)
        gs = jnp.where(jnp.arange(nb) < cur, gs, NEG)
        vals, idx = lax.top_k(gs, n_top)
        ok = vals > NEG * 0.5
        kg = k_blk[bi, hi, idx]
        vg = v_blk[bi, hi, idx]
        s_sel = jnp.einsum('bqhd,bhqnld->bhqnl', qc, kg).reshape(b, MOBA_HEADS, Q_CHUNK, n_top * MOBA_BLOCK)
        m_sel = jnp.broadcast_to(ok[..., None], (b, MOBA_HEADS, Q_CHUNK, n_top, MOBA_BLOCK)).reshape(b, MOBA_HEADS, Q_CHUNK, n_top * MOBA_BLOCK)
        k_own = lax.dynamic_slice_in_dim(k_pad, cur * MOBA_BLOCK, MOBA_BLOCK, 1)
        v_own = lax.dynamic_slice_in_dim(v_pad, cur * MOBA_BLOCK, MOBA_BLOCK, 1)
        opos = cur * MOBA_BLOCK + jnp.arange(MOBA_BLOCK)
        s_own = jnp.einsum('bqhd,blhd->bhql', qc, k_own)
        m_own = jnp.broadcast_to(opos[None, :] <= t[:, None], (b, MOBA_HEADS, Q_CHUNK, MOBA_BLOCK))
        scores = jnp.concatenate([s_sel, s_own], axis=-1).astype(jnp.float32) * scale
        p = masked_softmax(scores, jnp.concatenate([m_sel, m_own], axis=-1)).astype(v.dtype)
        p_sel = p[..., :n_top * MOBA_BLOCK].reshape(b, MOBA_HEADS, Q_CHUNK, n_top, MOBA_BLOCK)
        p_own = p[..., n_top * MOBA_BLOCK:]
        return jnp.einsum('bhqnl,bhqnld->bqhd', p_sel, vg) + jnp.einsum('bhql,blhd->bqhd', p_own, v_own)

    out = lax.map(chunk, jnp.arange(s // Q_CHUNK))
    return jnp.transpose(out, (1, 0, 2, 3, 4)).reshape(b, s, MOBA_W)


def token_mixer(h, w_in, cmp_pe, cmp_w1, cmp_w2, w_up_nsa, w_up_moba, w_out):
    b, s, _ = h.shape
    pos = jnp.arange(s, dtype=jnp.int32)
    proj = h @ w_in
    sizes = (NSA_Q, NSA_KV, NSA_KV, NSA_KV, NSA_KV, NSA_KV, NSA_KV, 3 * NSA_HEADS,
             MOBA_W, MOBA_W, MOBA_W, D_MODEL, D_MODEL)
    nq, kc, vc, ks, vs, kw, vw, ng, mq, mk, mv, gn, gm = split_cols(proj, sizes)
    nq = nq.reshape(b, s, NSA_HEADS, HEAD_DIM)
    kvh = lambda z: z.reshape(b, s, NSA_KV_HEADS, HEAD_DIM)
    mh = lambda z: z.reshape(b, s, MOBA_HEADS, HEAD_DIM)
    kc = nsa_compress(kvh(kc), cmp_pe[0], cmp_w1[0], cmp_w2[0])
    vc = nsa_compress(kvh(vc), cmp_pe[1], cmp_w1[1], cmp_w2[1])
    nsa_gates = jax.nn.sigmoid(ng).reshape(b, s, NSA_HEADS, 3)
    o_nsa = nsa_attention(partial_rope(nq, pos), nq, kc, vc, partial_rope(kvh(ks), pos), kvh(vs),
                          partial_rope(kvh(kw), pos), kvh(vw), nsa_gates)
    o_moba = moba_attention(partial_rope(mh(mq), pos), partial_rope(mh(mk), pos), mh(mv))
    y = jax.nn.sigmoid(gn) * (o_nsa @ w_up_nsa) + jax.nn.sigmoid(gm) * (o_moba @ w_up_moba)
    return y @ w_out


def peer_ffn(h, wq, k1, k2, u, v):
    b, s, d = h.shape
    half = PEER_QDIM // 2
    tokens = h.reshape(-1, PEER_CHUNK, d)

    def chunk(ht):
        q = (ht @ wq).reshape(PEER_CHUNK, PEER_HEADS, 2, half)
        s1 = jnp.einsum('thd,nd->thn', q[:, :, 0], k1).astype(jnp.float32)
        s2 = jnp.einsum('thd,nd->thn', q[:, :, 1], k2).astype(jnp.float32)
        v1, i1 = lax.top_k(s1, PEER_TOPK)
        v2, i2 = lax.top_k(s2, PEER_TOPK)
        cand = (v1[..., :, None] + v2[..., None, :]).reshape(PEER_CHUNK, PEER_HEADS, PEER_TOPK * PEER_TOPK)
        vals, ci = lax.top_k(cand, PEER_TOPK)
        e = (jnp.take_along_axis(i1, ci // PEER_TOPK, axis=-1) * PEER_NKEYS
             + jnp.take_along_axis(i2, ci % PEER_TOPK, axis=-1))
        gate = jax.nn.softmax(vals, axis=-1).astype(ht.dtype)
        act = jax.nn.gelu(jnp.einsum('td,thkd->thk', ht, u[e]))
        return jnp.einsum('thk,thkd->td', gate * act, v[e])

    return lax.map(chunk, tokens).reshape(b, s, d)


def setup_inputs(seed: int = 0) -> dict:
    key = jax.random.key(seed)
    ks = jax.random.split(key, 20)
    nrm = lambda k, shape, sc: jax.random.normal(k, shape, jnp.float32) * sc
    D = D_MODEL
    return {
        "x": nrm(ks[0], (BATCH, SEQ, D), 1.0),
        "c": nrm(ks[1], (BATCH, D), 1.0),
        "w_ada": nrm(ks[2], (DEPTH, D, 6 * D), 0.5 * D ** -0.5),
        "b_ada": nrm(ks[3], (DEPTH, 6 * D), 0.02),
        "g_attn": 1.0 + nrm(ks[4], (DEPTH, D), 0.02),
        "g_ffn": 1.0 + nrm(ks[5], (DEPTH, D), 0.02),
        "w_in": nrm(ks[6], (DEPTH, D, IN_COLS), D ** -0.5),
        "cmp_pe": nrm(ks[7], (DEPTH, 2, CMP_LEN, HEAD_DIM), 0.1),
        "cmp_w1": nrm(ks[8], (DEPTH, 2, CMP_LEN * HEAD_DIM, CMP_HID), (CMP_LEN * HEAD_DIM) ** -0.5),
        "cmp_w2": nrm(ks[9], (DEPTH, 2, CMP_HID, HEAD_DIM), CMP_HID ** -0.5),
        "w_up_nsa": nrm(ks[10], (DEPTH, NSA_Q, D), NSA_Q ** -0.5),
        "w_up_moba": nrm(ks[11], (DEPTH, MOBA_W, D), MOBA_W ** -0.5),
        "w_out": nrm(ks[12], (DEPTH, D, D), D ** -0.5),
        "peer_wq": nrm(ks[13], (DEPTH, D, PEER_HEADS * PEER_QDIM), D ** -0.5),
        "peer_k1": nrm(ks[14], (DEPTH, PEER_NKEYS, PEER_QDIM // 2), (PEER_QDIM // 2) ** -0.5),
        "peer_k2": nrm(ks[15], (DEPTH, PEER_NKEYS, PEER_QDIM // 2), (PEER_QDIM // 2) ** -0.5),
        "peer_u": nrm(ks[16], (DEPTH, PEER_EXPERTS, D), D ** -0.5),
        "peer_v": nrm(ks[17], (DEPTH, PEER_EXPERTS, D), PEER_HEADS ** -0.5),
        "g_final": 1.0 + nrm(ks[18], (D,), 0.02),
    }


def reference(x, c, w_ada, b_ada, g_attn, g_ffn, w_in, cmp_pe, cmp_w1, cmp_w2, w_up_nsa, w_up_moba,
              w_out, peer_wq, peer_k1, peer_k2, peer_u, peer_v, g_final):
    sc_c = jax.nn.silu(c)
    for l in range(DEPTH):
        mod = sc_c @ w_ada[l] + b_ada[l]
        sh1, sc1, ga1, sh2, sc2, ga2 = jnp.split(mod, 6, axis=-1)
        h = rms_norm(x, g_attn[l]) * (1.0 + sc1[:, None, :]) + sh1[:, None, :]
        x = x + ga1[:, None, :] * token_mixer(h, w_in[l], cmp_pe[l], cmp_w1[l], cmp_w2[l],
                                              w_up_nsa[l], w_up_moba[l], w_out[l])
        h = rms_norm(x, g_ffn[l]) * (1.0 + sc2[:, None, :]) + sh2[:, None, :]
        x = x + ga2[:, None, :] * peer_ffn(h, peer_wq[l], peer_k1[l], peer_k2[l], peer_u[l], peer_v[l])
    return rms_norm(x, g_final)
```

```python
import math
import numpy as np
import ml_dtypes
from contextlib import ExitStack
import concourse.bass as bass
import concourse.mybir as mybir
from concourse.bass_utils import run_bass_kernel_spmd

F32 = mybir.dt.float32
BF16 = mybir.dt.bfloat16
I32 = mybir.dt.int32
U32 = mybir.dt.uint32
AF = mybir.ActivationFunctionType
ALU = mybir.AluOpType
AX = mybir.AxisListType

NDMASEM = 24
SEMLIM = 1 << 28
COMPUTE = ("act", "dve", "pool", "pe")

D = 1024
NEG = -1.0e30
EPS = 1e-6
ROPE_THETA = 500000.0


class Buf:
    __slots__ = ("name", "t", "last_w", "readers", "excl", "parent")

    def __init__(self, name, t=None, excl=False, parent=None):
        self.name = name
        self.t = t
        self.last_w = None
        self.readers = []
        self.excl = excl
        self.parent = parent

    def view(self, name, ap):
        return Buf(name, ap, excl=self.excl, parent=self if self.parent is None else self.parent)

    def __getitem__(self, k):
        return self.t[k]


class Sched:
    def __init__(self, nc, stack):
        self.nc = nc
        self.stack = stack
        self.outer = stack
        self.ops = []
        self.pfx = ""
        self.emitted = 0
        self.cnt = {e: 0 for e in COMPUTE}
        self.ndma = 0
        self.sems = {}
        self.last_eng = {}
        self.recent_dma = []

    def sbuf(self, name, shape, dtype):
        t = self.stack.enter_context(self.nc.sbuf_tensor("sb_" + self.pfx + name, list(shape), dtype))
        return Buf(name, t)

    def psum(self, name, shape, dtype):
        t = self.stack.enter_context(self.nc.psum_tensor("ps_" + self.pfx + name, list(shape), dtype))
        return Buf(name, t, excl=True)

    def din(self, name, shape, dtype):
        return Buf(name, self.nc.dram_tensor(name, list(shape), dtype, kind="ExternalInput"))

    def dout(self, name, shape, dtype):
        return Buf(name, self.nc.dram_tensor(name, list(shape), dtype, kind="ExternalOutput"))

    def dscr(self, name, shape, dtype):
        return Buf(name, self.nc.dram_tensor(name, list(shape), dtype, kind="Internal"))

    def op(self, eng, fn, reads=(), writes=(), dma=False):
        i = len(self.ops)
        deps = set()
        reads = [b if b.parent is None else b.parent for b in reads]
        writes = [b if b.parent is None else b.parent for b in writes]
        ex = [b for b in reads if b.excl and b not in writes]
        if ex:
            writes = list(writes) + ex
        for b in reads:
            if b.last_w is not None:
                deps.add(b.last_w)
        for b in writes:
            if b.last_w is not None:
                deps.add(b.last_w)
            for r in b.readers:
                deps.add(r)
        deps.discard(i)
        for b in reads:
            b.readers.append(i)
        for b in writes:
            b.last_w = i
            b.readers = []
        self.ops.append(dict(eng=eng, fn=fn, deps=deps, dma=dma, signal=False))
        if dma:
            self.recent_dma = (self.recent_dma + [i])[-NDMASEM:]
        else:
            self.last_eng[eng] = i
        return i

    def barrier(self):
        deps = set(self.last_eng.values()) | set(self.recent_dma)
        for e in ("sp", "act", "dve", "pool", "pe"):
            i = self.op(e, lambda eng: eng.nop(), (), ())
            self.ops[i]["deps"] |= deps

    def dma(self, out, in_, reads=(), writes=(), eng="sp", **kw):
        return self.op(eng, lambda e: e.dma_start(out=out, in_=in_, **kw), reads, writes, dma=True)

    def mm(self, out, lhsT, rhs, start, stop, reads, writes):
        return self.op("pe", lambda e: e.matmul(out, lhsT=lhsT, rhs=rhs, start=start, stop=stop,
                                                skip_group_check=True), reads, writes)

    def tr(self, out, in_, ident, reads, writes):
        return self.op("pe", lambda e: e.transpose(out, in_, ident), reads, writes)

    def act(self, out, in_, func, reads, writes, **kw):
        return self.op("act", lambda e: e.activation(out=out, in_=in_, func=func, **kw), reads, writes)

    def tt(self, out, in0, in1, op, reads, writes, eng="dve"):
        return self.op(eng, lambda e: e.tensor_tensor(out=out, in0=in0, in1=in1, op=op), reads, writes)

    def ts(self, out, in0, s1, s2, op0, op1, reads, writes, eng="dve", **kw):
        if op1 is None:
            return self.op(eng, lambda e: e.tensor_scalar(out=out, in0=in0, scalar1=s1, scalar2=None,
                                                          op0=op0, **kw), reads, writes)
        return self.op(eng, lambda e: e.tensor_scalar(out=out, in0=in0, scalar1=s1, scalar2=s2,
                                                      op0=op0, op1=op1, **kw), reads, writes)

    def stt(self, out, in0, scalar, in1, op0, op1, reads, writes, **kw):
        return self.op("dve", lambda e: e.scalar_tensor_tensor(out=out, in0=in0, scalar=scalar, in1=in1,
                                                               op0=op0, op1=op1, **kw), reads, writes)

    def copy(self, out, in_, reads, writes, eng="dve"):
        if eng == "act":
            return self.op("act", lambda e: e.copy(out=out, in_=in_), reads, writes)
        return self.op(eng, lambda e: e.tensor_copy(out=out, in_=in_), reads, writes)

    def memset(self, ap, val, writes, eng="pool"):
        return self.op(eng, lambda e: e.memset(ap, val), (), writes)

    def emit_block(self, stack):
        self.barrier()
        nc = self.nc
        ops = self.ops
        lo, hi = self.emitted, len(ops)
        for i in range(lo, hi):
            ops[i]["deps"] = {d for d in ops[i]["deps"] if d >= lo}
        for i in range(lo, hi):
            o = ops[i]
            for d in o["deps"]:
                po = ops[d]
                if po["eng"] == "pe" and o["eng"] == "pe" and not po["dma"] and not o["dma"]:
                    continue
                po["signal"] = True
        per_gen = NDMASEM * (SEMLIM // 16)
        for i in range(lo, hi):
            o = ops[i]
            if o["dma"]:
                g = self.ndma // per_gen
                r = self.ndma % per_gen
                o["sem"] = ("dma", r % NDMASEM, g)
                o["tgt"] = 16 * (r // NDMASEM + 1)
                self.ndma += 1
            elif o["signal"]:
                g = self.cnt[o["eng"]] // SEMLIM
                o["sem"] = ("c", o["eng"], g)
                o["tgt"] = self.cnt[o["eng"]] % SEMLIM + 1
                self.cnt[o["eng"]] += 1
            else:
                continue
            if o["sem"] not in self.sems:
                k = o["sem"]
                self.sems[k] = self.outer.enter_context(nc.semaphore("s_" + "_".join(str(x) for x in k)))
        sems = self.sems
        block = stack.enter_context(nc.Block())
        engs = ["sp", "act", "dve", "pool", "pe"]
        per = {e: [] for e in engs}
        for i in range(lo, hi):
            per[ops[i]["eng"]].append(i)

        def run(ename, eobj):
            known = {}
            for i in per[ename]:
                o = ops[i]
                waits = {}
                for d in o["deps"]:
                    po = ops[d]
                    if (not po["dma"]) and (not o["dma"]) and po["eng"] == "pe" and ename == "pe":
                        continue
                    if not (po["dma"] or po["signal"]):
                        continue
                    k = po["sem"]
                    waits[k] = max(waits.get(k, 0), po["tgt"])
                if o["dma"]:
                    k = o["sem"]
                    prev = o["tgt"] - 16
                    if prev > 0:
                        waits[k] = max(waits.get(k, 0), prev)
                for k, v in waits.items():
                    if known.get(k, 0) >= v:
                        continue
                    eobj.wait_ge(sems[k], v)
                    known[k] = v
                ins = o["fn"](eobj)
                if o["dma"]:
                    ins.then_inc(sems[o["sem"]], 16)
                elif o["signal"]:
                    ins.then_inc(sems[o["sem"]], 1)
                o["fn"] = None

        @block.sync
        def _(e):
            run("sp", e)

        @block.scalar
        def _(e):
            run("act", e)

        @block.vector
        def _(e):
            run("dve", e)

        @block.gpsimd
        def _(e):
            run("pool", e)

        @block.tensor
        def _(e):
            run("pe", e)

        self.emitted = hi


def make_ident(S, dtype, name):
    ones = S.sbuf(name + "_ones", [128, 128], F32)
    idt = S.sbuf(name, [128, 128], dtype)
    S.memset(ones[:], 1.0, [ones])
    S.op("pool", lambda e: e.affine_select(out=idt[:], in_=ones[:], pattern=[[1, 128]],
                                           compare_op=ALU.is_equal, fill=0.0, base=0,
                                           channel_multiplier=-1), [ones], [idt])
    return idt


def load_cast_weight(S, dst, dst_col0, src, ncols, stage, K=8):
    c0 = 0
    while c0 < ncols:
        n = min(512, ncols - c0)
        S.dma(stage[:, :, 0:n], src.t[:, c0:c0 + n].rearrange("(k p) n -> p k n", p=128), writes=[stage])
        S.copy(dst[:, :, dst_col0 + c0:dst_col0 + c0 + n], stage[:, :, 0:n], [stage], [dst], eng="pool")
        c0 += n


def compute_mod(S, cT_d, wada_d, bada_d, ncols, name, wst, ps=None):
    cT = S.sbuf(name + "_cT", [128, 8], F32)
    S.dma(cT[:], cT_d.t[:, :], writes=[cT])
    sT = S.sbuf(name + "_sT", [128, 8], F32)
    S.act(sT[:], cT[:], AF.Silu, [cT], [sT])
    lhs = S.sbuf(name + "_lhs", [128, 8, 128], F32)
    for k in range(8):
        S.copy(lhs[:, k, :], sT[:, k:k + 1].to_broadcast([128, 128]), [sT], [lhs])
    mod = S.sbuf(name, [128, ncols], F32)
    bst = S.sbuf(name + "_bst", [128, 512], F32)
    if ps is None:
        ps = S.psum(name + "_ps", [128, 512], F32)
    for c0 in range(0, ncols, 512):
        S.dma(wst[:], wada_d.t[:, c0:c0 + 512].rearrange("(k p) n -> p k n", p=128), writes=[wst])
        S.dma(bst[:], bada_d.t[:, c0:c0 + 512], writes=[bst])
        for k in range(8):
            S.mm(ps[:], lhs[:, k, :], wst[:, k, :], k == 0, k == 7, [lhs, wst], [ps])
        S.tt(mod[:, c0:c0 + 512], ps[:], bst[:], ALU.add, [ps, bst], [mod])
    return mod


def rmsnorm_mod_tile(S, x_ap, xbuf, A, Bm, out_ap, outbuf, scr, small, Bbuf=None):
    S.act(scr[:], x_ap, AF.Square, [xbuf], [scr, small], accum_out=small[:, 0:1])
    S.ts(small[:, 1:2], small[:, 0:1], 1.0 / D, EPS, ALU.mult, ALU.add, [small], [small])
    S.act(small[:, 2:3], small[:, 1:2], AF.Sqrt, [small], [small])
    S.op("dve", lambda e: e.reciprocal(out=small[:, 3:4], in_=small[:, 2:3]), [small], [small])
    if Bm is None:
        S.stt(out_ap, x_ap, small[:, 3:4], A[:], ALU.mult, ALU.mult, [xbuf, small, A], [outbuf])
    else:
        S.stt(scr[:], x_ap, small[:, 3:4], A[:], ALU.mult, ALU.mult, [xbuf, small, A], [scr])
        S.tt(out_ap, scr[:], Bm, ALU.add, [scr, Bbuf], [outbuf])


ROTENG = "dve"
NHT = 32
ROPE_IDX = list(range(0, 8)) + [12, 13, 14, 15] + list(range(16, 32))
NM = 792


def build_stageA(TL, level=99, S=None, io=None, pfx=""):
    alone = S is None
    nc = bass.Bass("TRN2", target_bir_lowering=False) if alone else S.nc
    NCH = TL // 512
    with ExitStack() as st:
        if alone:
            S = Sched(nc, st)
        S.stack = st
        S.pfx = pfx
        _in = (lambda n, sh, dt: S.din(n, sh, dt)) if alone else (lambda n, sh, dt: io[n])
        _out = (lambda n, sh, dt: S.dout(n, sh, dt)) if alone else (lambda n, sh, dt: io[n])
        xs = _in("xs", [TL, D], F32)
        cT_d = _in("cT", [128, 8], F32)
        wada = _in("wada", [D, 2048], F32)
        bada = _in("bada", [128, 2048], F32)
        gat = _in("gattn", [128, D], F32)
        wT_d = _in("wT", [D, NHT * 64], F32)
        wS_d = _in("wS", [D, 28 * 64], F32)
        wM_d = _in("wM", [D, NM], F32)
        pos_d = _in("pos", [16, TL], F32)
        oT = _out("oT", [NHT, 64, TL // 2], F32)
        oQ = _out("oQ", [8, 64, TL // 2], F32)
        oM = _out("oM", [TL, 384], F32)
        oG = _out("oG", [TL, 24], F32)
        oK = _out("oK", [8, 64, TL // 256], F32)

        identb = make_ident(S, BF16, "identb")
        wT = S.sbuf("wTb", [128, 8, NHT * 64], BF16)
        wS = S.sbuf("wSb", [128, 8, 28 * 64], BF16)
        wM = S.sbuf("wMb", [128, 8, NM], BF16)
        stage = S.sbuf("wstage", [128, 8, 512], F32)
        load_cast_weight(S, wT, 0, wT_d, NHT * 64, stage)
        load_cast_weight(S, wS, 0, wS_d, 28 * 64, stage)
        load_cast_weight(S, wM, 0, wM_d, NM, stage)
        if level <= 1:
            S.emit_block(st); return nc
        mod = compute_mod(S, cT_d, wada, bada, 2048, "modA", stage)
        gt = S.sbuf("gat", [128, D], F32)
        S.dma(gt[:], gat.t[:, :], writes=[gt])
        A1 = S.sbuf("A1", [128, D], F32)
        S.stt(A1[:], mod[:, 1024:2048], 1.0, gt[:], ALU.add, ALU.mult, [mod, gt], [A1])
        B1 = mod[:, 0:1024]
        if level <= 2:
            S.emit_block(st); return nc
        pidx_i = S.sbuf("pidx_i", [16, 1], I32)
        S.op("pool", lambda e: e.iota(pidx_i[:], pattern=[[0, 1]], base=0, channel_multiplier=1), [], [pidx_i])
        pf = S.sbuf("pf", [16, 4], F32)
        S.copy(pf[:, 0:1], pidx_i[:], [pidx_i], [pf])
        S.ts(pf[:, 3:4], pf[:, 0:1], 8.0, 2.0, ALU.is_ge, ALU.mult, [pf], [pf])
        S.stt(pf[:, 1:2], pf[:, 3:4], -4.0, pf[:, 0:1], ALU.mult, ALU.add, [pf], [pf])
        S.ts(pf[:, 3:4], pf[:, 3:4], -1.0, None, ALU.add, None, [pf], [pf])
        S.act(pf[:, 2:3], pf[:, 1:2], AF.Exp, [pf], [pf], scale=-math.log(ROPE_THETA) / 8.0)
        S.ts(pf[:, 2:3], pf[:, 2:3], 1.0 / (2 * math.pi), None, ALU.mult, None, [pf], [pf])

        if level <= 3:
            S.emit_block(st); return nc
        xt = [S.sbuf("xt%d" % i, [128, D], F32) for i in range(2)]
        scr = S.sbuf("scr", [128, D], F32)
        small = S.sbuf("small", [128, 4], F32)
        hb = S.sbuf("hb", [128, D], BF16)
        hT = S.sbuf("hT", [128, 8, 512], BF16)
        pstr = S.psum("pstr", [128, 8, 128], BF16)
        psh = [S.psum("psh%d" % i, [64, 512], F32) for i in range(2)]
        pss = [S.psum("pss%d" % i, [64, 512], F32) for i in range(2)]
        psm = [S.psum("psm%d" % i, [128, 512], F32) for i in range(2)]
        outT = S.sbuf("outT", [64, NHT, 512], BF16)
        outQ = S.sbuf("outQ", [64, 8, 512], BF16)
        outM = S.sbuf("outM", [128, 768], BF16)
        outG = S.sbuf("outG", [128, 24], F32)
        km = S.sbuf("km", [64, 8, 2], F32)
        posr = S.sbuf("posr", [16, 512], F32)
        ry = S.sbuf("ry", [16, 512], F32)
        ryi = S.sbuf("ryi", [16, 512], I32)
        ryf = S.sbuf("ryf", [16, 512], F32)
        C16 = S.sbuf("C16", [16, 512], F32)
        S16 = S.sbuf("S16", [16, 512], F32)
        t1 = S.sbuf("t1", [16, 512], F32)
        t2 = S.sbuf("t2", [16, 512], F32)
        rot = S.sbuf("rot", [16, 512], F32)

        def frac_sin(dst, add):
            S.ts(ryf[:], ry[:], add, None, ALU.add, None, [ry], [ryf])
            S.copy(ryi[:], ryf[:], [ryf], [ryi])
            S.copy(t1[:], ryi[:], [ryi], [t1])
            S.tt(t2[:], ryf[:], t1[:], ALU.subtract, [ryf, t1], [t2])
            S.stt(t1[:], t2[:], 0.5, t2[:], ALU.is_gt, ALU.subtract, [t2], [t1])
            S.stt(t2[:], t1[:], 0.5, t1[:], ALU.is_gt, ALU.subtract, [t1], [t2])
            S.act(dst[:], t2[:], AF.Sin, [t2], [dst], scale=2 * math.pi)

        for ch in range(NCH):
            t0 = ch * 512
            S.dma(posr[:], pos_d.t[:, t0:t0 + 512], writes=[posr])
            S.ts(ry[:], posr[:], pf[:, 2:3], None, ALU.mult, None, [posr, pf], [ry])
            frac_sin(S16, 0.0)
            S.ts(S16[:], S16[:], pf[:, 3:4], None, ALU.mult, None, [S16, pf], [S16])
            frac_sin(C16, 0.25)
            if level <= 4:
                continue
            for tt_ in range(4):
                xb = xt[tt_ % 2]
                S.dma(xb[:], xs.t[t0 + tt_ * 128:t0 + (tt_ + 1) * 128, :], reads=[xs], writes=[xb])
                rmsnorm_mod_tile(S, xb[:], xb, A1, B1, hb[:], hb, scr, small, Bbuf=mod)
                for k in range(8):
                    S.tr(pstr[:, k, :], hb[:, k * 128:(k + 1) * 128], identb[:], [hb, identb], [pstr])
                S.copy(hT[:, :, tt_ * 128:(tt_ + 1) * 128], pstr[:], [pstr], [hT], eng="act")
                for half, (c0, c1) in enumerate(((0, 512), (512, NM))):
                    pm = psm[half]
                    for k in range(8):
                        S.mm(pm[:, 0:c1 - c0], hT[:, k, tt_ * 128:(tt_ + 1) * 128], wM[:, k, c0:c1],
                             k == 0, k == 7, [hT, wM], [pm])
                S.copy(outM[:, 0:512], psm[0][:, 0:512], [psm[0]], [outM], eng="act")
                S.copy(outM[:, 512:768], psm[1][:, 0:256], [psm[1]], [outM], eng="act")
                S.act(outG[:], psm[1][:, 256:280], AF.Sigmoid, [psm[1]], [outG])
                S.dma(oM.t[t0 + tt_ * 128:t0 + (tt_ + 1) * 128, :], outM[:].bitcast(F32), reads=[outM], writes=[oM])
                S.dma(oG.t[t0 + tt_ * 128:t0 + (tt_ + 1) * 128, :], outG[:], reads=[outG], writes=[oG])
            for i in range(NHT if level > 5 else 0):
                ph = psh[i % 2]
                for k in range(8):
                    S.mm(ph[:], wT[:, k, i * 64:(i + 1) * 64], hT[:, k, :], k == 0, k == 7, [wT, hT], [ph])
                S.copy(outT[:, i, :], ph[:], [ph], [outT], eng="act")
                if i < 8:
                    S.copy(outQ[:, i, :], ph[:], [ph], [outQ], eng="act")
                if i >= 24 and level > 7:
                    S.op("dve", lambda e, ph=ph, i=i: e.tensor_reduce(
                        out=km[:, i - 24, :], in_=ph[:].rearrange("p (b t) -> p b t", t=256),
                        axis=AX.X, op=ALU.add), [ph], [km])
                if i in ROPE_IDX and level > 6.05:
                    r = ROPE_IDX.index(i)
                    p2 = pss[r % 2]
                    for k in range(8):
                        S.mm(p2[:], wS[:, k, r * 64:(r + 1) * 64], hT[:, k, :], k == 0, k == 7, [wS, hT], [p2])
                    if level > 6.15:
                        S.tt(t1[:], ph[0:16, :], C16[:], ALU.mult, [ph, C16], [t1])
                    if level > 6.25:
                        S.tt(t2[:], p2[0:16, :], S16[:], ALU.mult, [p2, S16], [t2])
                    if level > 6.35:
                        S.tt(rot[:], t1[:], t2[:], ALU.add, [t1, t2], [rot])
                    if level > 6.45:
                        S.copy(outT[0:16, i, :], rot[:], [rot], [outT], eng=ROTENG)
                    if i >= 24 and level > 7:
                        S.op("dve", lambda e, i=i: e.tensor_reduce(
                            out=km[0:16, i - 24, :], in_=rot[:].rearrange("p (b t) -> p b t", t=256),
                            axis=AX.X, op=ALU.add), [rot], [km])
            if level > 7:
                S.ts(km[:], km[:], 1.0 / 256.0, None, ALU.mult, None, [km], [km])
            S.dma(oT.t[:, :, t0 // 2:t0 // 2 + 256].rearrange("i d t -> d i t"), outT[:].bitcast(F32), reads=[outT], writes=[oT])
            S.dma(oQ.t[:, :, t0 // 2:t0 // 2 + 256].rearrange("i d t -> d i t"), outQ[:].bitcast(F32), reads=[outQ], writes=[oQ])
            if level > 7:
                S.dma(oK.t[:, :, ch * 2:ch * 2 + 2].rearrange("h d b -> d h b"), km[:], reads=[km], writes=[oK])
        S.emit_block(st)
        S.stack = S.outer
    return nc


C_NQ, C_KC, C_VC, C_KS, C_VS, C_KW, C_VW, C_NG, C_MQ, C_MK, C_MV, C_GN, C_GM = (
    0, 512, 640, 768, 896, 1024, 1152, 1280, 1304, 1816, 2328, 2840, 3864)
HT_COLS = ([C_NQ + 64 * h for h in range(8)] + [C_KC, C_KC + 64] + [C_VC, C_VC + 64] + [C_KS, C_KS + 64]
           + [C_KW, C_KW + 64] + [C_MQ + 64 * h for h in range(8)] + [C_MK + 64 * h for h in range(8)])


def _f32(a):
    return np.ascontiguousarray(a, dtype=np.float32)


def host_inputs_A(inp, l, x_cur, S):
    TL = S // 4
    w_in = inp["w_in"][l]
    wT = np.concatenate([w_in[:, c:c + 64] for c in HT_COLS], axis=1)
    sw = []
    for i in ROPE_IDX:
        c = HT_COLS[i]
        sw.append(w_in[:, c + 8:c + 16])
        sw.append(w_in[:, c:c + 8])
        sw.append(np.zeros((D, 48), np.float32))
    wS = np.concatenate(sw, axis=1)
    wM = np.concatenate([w_in[:, C_VS:C_VS + 128], w_in[:, C_VW:C_VW + 128], w_in[:, C_MV:C_MV + 512],
                         w_in[:, C_NG:C_NG + 24]], axis=1)
    wada = inp["w_ada"][l][:, 0:2048]
    bada = np.broadcast_to(inp["b_ada"][l][None, 0:2048], (128, 2048))
    gat = np.broadcast_to(inp["g_attn"][l][None, :], (128, D))
    maps = []
    for cid in range(8):
        b, g = cid // 4, cid % 4
        pos = np.broadcast_to(np.arange(g * TL, (g + 1) * TL, dtype=np.float32)[None, :], (16, TL))
        maps.append({
            "xs": _f32(x_cur[b, g * TL:(g + 1) * TL]),
            "cT": _f32(inp["c"][b].reshape(8, 128).T),
            "wada": _f32(wada), "bada": _f32(bada), "gattn": _f32(gat),
            "wT": _f32(wT), "wS": _f32(wS), "wM": _f32(wM), "pos": _f32(pos),
        })
    return maps


SCALE = 0.125
MASKBIG = 240000.0
GELU_C = 1.5957691216057308


def gelu_tanh(S, out_ap, outbuf, x, sq, tmp, n):
    S.act(sq[:, 0:n], x[:, 0:n], AF.Square, [x], [sq])
    S.ts(sq[:, 0:n], sq[:, 0:n], 0.044715, 1.0, ALU.mult, ALU.add, [sq], [sq])
    S.tt(tmp[:, 0:n], sq[:, 0:n], x[:, 0:n], ALU.mult, [sq, x], [tmp])
    S.act(sq[:, 0:n], tmp[:, 0:n], AF.Sigmoid, [tmp], [sq], scale=GELU_C)
    S.tt(out_ap, x[:, 0:n], sq[:, 0:n], ALU.mult, [x, sq], [outbuf])


def build_stageC(SQ, S=None, io=None, pfx="", g=None):
    alone = S is None
    nc = bass.Bass("TRN2", target_bir_lowering=False) if alone else S.nc
    NQC = SQ // 512
    NKT = SQ // 128
    NB = SQ // 256
    NCMP = SQ // 16 - 1
    NCT = (NCMP + 127) // 128
    with ExitStack() as st:
        if alone:
            S = Sched(nc, st)
        S.stack = st
        S.pfx = pfx
        _in = (lambda n, sh, dt: S.din(n, sh, dt)) if alone else (lambda n, sh, dt: io[n])
        _out = (lambda n, sh, dt: S.dout(n, sh, dt)) if alone else (lambda n, sh, dt: io[n])
        if alone:
            qraw_d = _in("qraw", [4, 64, SQ // 2], F32)
            qrot_d = _in("qrot", [2, 64, SQ // 2], F32)
            kcp_d = _in("kcp", [2, 64, SQ // 2], F32)
            ks_d = _in("ksT", [64, SQ // 2], F32)
            kw_d = _in("kwT", [64, SQ // 2], F32)
            vsw_d = _in("vsw", [SQ, 64], F32)
            gates_d = _in("gates", [SQ, 6], F32)
            mq_d = _in("mq", [2, 64, SQ // 2], F32)
            mk_d = _in("mk", [2, 64, SQ // 2], F32)
            mv_d = _in("mv", [SQ, 64], F32)
            km_d = _in("kmean", [2, 64, NB], F32)
            onsa_d = _out("onsa", [SQ, 64], F32)
            omoba_d = _out("omoba", [SQ, 64], F32)
            qrawA = qraw_d.view("qrawA", qraw_d.t[0:2])
            qrawB = qraw_d.view("qrawB", qraw_d.t[2:4])
            vs_d = vsw_d.view("vs_d", vsw_d.t[:, 0:32])
            vw_d = vsw_d.view("vw_d", vsw_d.t[:, 32:64])
        else:
            kv = g // 2
            oth0 = 4 * kv + (2 if g % 2 == 0 else 0)
            oT_, oQ_, oM_, oG_, oK_ = io["oT"], io["oQ"], io["oM"], io["oG"], io["oK"]
            qrawA = oQ_.view("qrawA", oQ_.t[2 * g:2 * g + 2])
            qrawB = oQ_.view("qrawB", oQ_.t[oth0:oth0 + 2])
            qrot_d = oT_.view("qrot", oT_.t[2 * g:2 * g + 2])
            kcp_d = oT_.view("kcp", oT_.t[8 + kv:12:2])
            ks_d = oT_.view("ksT", oT_.t[12 + kv])
            kw_d = oT_.view("kwT", oT_.t[14 + kv])
            vs_d = oM_.view("vs_d", oM_.t[:, 32 * kv:32 * kv + 32])
            vw_d = oM_.view("vw_d", oM_.t[:, 64 + 32 * kv:64 + 32 * kv + 32])
            gates_d = oG_.view("gates", oG_.t[:, 6 * g:6 * g + 6])
            mq_d = oT_.view("mq", oT_.t[16 + 2 * g:18 + 2 * g])
            mk_d = oT_.view("mk", oT_.t[24 + 2 * g:26 + 2 * g])
            mv_d = oM_.view("mv", oM_.t[:, 128 + 64 * g:128 + 64 * g + 64])
            km_d = oK_.view("kmean", oK_.t[2 * g:2 * g + 2])
            onsa_d = io["oN"].view("onsa", io["oN"].t[:, 64 * g:64 * g + 64])
            omoba_d = io["oMo"].view("omoba", io["oMo"].t[:, 64 * g:64 * g + 64])
        peT_d = _in("peT", [2, 64, 32], F32)
        w1_d = _in("w1", [2, 2048, 128], F32)
        w2_d = _in("w2", [2, 128, 64], F32)

        identf = make_ident(S, F32, "identf")
        identb = make_ident(S, BF16, "identb")
        PB = [S.psum("PB%d" % i, [128, 512], F32) for i in range(8)]

        onesb = S.sbuf("onesb", [128, 1024], BF16)
        S.memset(onesb[:], 1.0, [onesb])
        Ex = S.sbuf("Ex", [128, 64, 128], BF16)
        for e0 in range(0, 64, 8):
            S.op("pool", lambda e, e0=e0: e.affine_select(
                out=Ex[:, e0:e0 + 8, :], in_=onesb[:].rearrange("p (a k) -> p a k", k=128),
                pattern=[[-2, 8], [-1, 2], [0, 64]], compare_op=ALU.is_equal, fill=0.0,
                base=-2 * e0, channel_multiplier=1), [onesb], [Ex])
        Rm = S.sbuf("Rm", [64, 64, 128], BF16)
        for e0 in range(0, 64, 8):
            S.op("pool", lambda e, e0=e0: e.affine_select(
                out=Rm[:, e0:e0 + 8, :], in_=onesb[0:64, :].rearrange("p (a k) -> p a k", k=128),
                pattern=[[-1, 8], [0, 128]], compare_op=ALU.is_equal, fill=0.0,
                base=-e0, channel_multiplier=1), [onesb], [Rm])
        vcx = S.sbuf("vcx", [128, NCT, 321], BF16)
        S.memset(vcx[:], 0.0, [vcx])
        S.memset(vcx[:, :, 320:321], 1.0, [vcx])
        ia = S.sbuf("ia", [128, 256], I32)
        ib = S.sbuf("ib", [128, 256], I32)
        fa = S.sbuf("fa", [128, 256], F32)
        fb = S.sbuf("fb", [128, 256], F32)
        fc = S.sbuf("fc", [128, 256], F32)
        fd = S.sbuf("fd", [128, 256], F32)
        S.op("pool", lambda e: e.iota(ib[:], pattern=[[64, 256]], base=0, channel_multiplier=0), [], [ib])
        S.copy(fb[:], ib[:], [ib], [fb])
        for ct in range(NCT):
            S.op("pool", lambda e, ct=ct: e.iota(ia[:], pattern=[[0, 256]], base=2048 * ct, channel_multiplier=16), [], [ia])
            S.copy(fa[:], ia[:], [ia], [fa])
            S.ts(fc[:], fa[:], 32.0, None, ALU.add, None, [fa], [fc])
            S.stt(fc[:], fb[:], 64.0, fc[:], ALU.add, ALU.min, [fb, fc], [fc])
            S.tt(fd[:], fa[:], fb[:], ALU.max, [fa, fb], [fd])
            S.tt(fc[:], fc[:], fd[:], ALU.subtract, [fc, fd], [fc])
            S.ts(vcx[:, ct, 0:256], fc[:], 0.0, 1.0 / 32.0, ALU.max, ALU.mult, [fc], [vcx])

        kcT = S.sbuf("kcT", [64, NCT * 128], BF16)
        S.memset(kcT[:], 0.0, [kcT])
        w1b = S.sbuf("w1b", [64, 2, 32, 128], BF16)
        w1st = S.sbuf("w1st", [64, 32, 128], F32)
        w2b = S.sbuf("w2b", [128, 2, 64], BF16)
        w2st = S.sbuf("w2st", [128, 2, 64], F32)
        peTb = S.sbuf("peTb", [64, 2, 32], BF16)
        peTst = S.sbuf("peTst", [64, 2, 32], F32)
        for j in range(2):
            S.dma(w1st[:], w1_d.t[j].rearrange("(l d) h -> d l h", d=64), writes=[w1st])
            S.copy(w1b[:, j, :, :], w1st[:], [w1st], [w1b], eng="pool")
        S.dma(w2st[:], w2_d.t[:, :, :].rearrange("j h d -> h j d"), writes=[w2st])
        S.copy(w2b[:], w2st[:], [w2st], [w2b])
        S.dma(peTst[:], peT_d.t[:, :, :].rearrange("j d l -> d j l"), writes=[peTst])
        S.copy(peTb[:], peTst[:], [peTst], [peTb])
        CCH = min(512, NCT * 128)
        kcp = S.sbuf("kcps", [64, 16 * CCH + 16], BF16)
        S.memset(kcp[:], 0.0, [kcp])
        hx = S.sbuf("hx", [128, CCH], F32)
        hsq = S.sbuf("hsq", [128, CCH], F32)
        htmp = S.sbuf("htmp", [128, CCH], F32)
        hg = S.sbuf("hg", [128, CCH], BF16)
        hbias = S.sbuf("hbias", [128, 1], F32)
        for j in range(2):
            for l in range(32):
                S.mm(PB[1][:, 0:1], w1b[:, j, l, :], peTb[:, j, l:l + 1], l == 0, l == 31, [w1b, peTb], [PB[1]])
            S.copy(hbias[:], PB[1][:, 0:1], [PB[1]], [hbias])
            for c0 in range(0, NCMP, CCH):
                n = min(CCH, NCMP - c0)
                ntok = 16 * (n - 1) + 32
                S.dma(kcp[:, 0:ntok].bitcast(F32), kcp_d.t[j, :, 8 * c0:8 * c0 + ntok // 2], reads=[kcp_d], writes=[kcp])
                for l in range(32):
                    S.mm(PB[0][:, 0:n], w1b[:, j, l, :], kcp[:, l:l + 16 * (n - 1) + 1:16], l == 0, l == 31,
                         [w1b, kcp], [PB[0]])
                if n < CCH:
                    S.memset(hg[:], 0.0, [hg])
                S.act(hx[:, 0:n], PB[0][:, 0:n], AF.Identity, [PB[0], hbias], [hx], bias=hbias[:, 0:1])
                gelu_tanh(S, hg[:, 0:n], hg, hx, hsq, htmp, n)
                if j == 0:
                    S.mm(PB[2][0:64, 0:n], w2b[:, 0, :], hg[:, 0:n], True, True, [w2b, hg], [PB[2]])
                    S.copy(kcT[:, c0:c0 + n], PB[2][0:64, 0:n], [PB[2]], [kcT], eng="act")
                else:
                    for tt_ in range((n + 127) // 128):
                        S.mm(PB[2][:, 0:64], hg[:, tt_ * 128:(tt_ + 1) * 128], w2b[:, 1, :], True, True, [hg, w2b], [PB[2]])
                        S.copy(vcx[:, c0 // 128 + tt_, 256:320], PB[2][:, 0:64], [PB[2]], [vcx], eng="act")
        if NCMP % 128:
            pass

        kmst = S.sbuf("kmst", [64, 2, NB], F32)
        kmb = S.sbuf("kmb", [64, 2, NB], BF16)
        S.dma(kmst[:], km_d.t[:, :, :].rearrange("h d b -> d h b"), reads=[km_d], writes=[kmst])
        S.copy(kmb[:], kmst[:], [kmst], [kmb])

        qraw = S.sbuf("qraw", [64, 4, 512], BF16)
        qrot = S.sbuf("qrot", [64, 2, 512], BF16)
        mq = S.sbuf("mqs", [64, 2, 512], BF16)
        gts = S.sbuf("gts", [128, 4, 6], F32)
        Eb = [S.sbuf("Eb%d" % i, [128, 512], BF16) for i in range(4)]
        Pb = [S.sbuf("Pb%d" % i, [128, 512], BF16) for i in range(2)]
        imp = S.sbuf("imp", [128, 4, 256], F32)
        sc = S.sbuf("sc", [128, 256], F32)
        sc2 = S.sbuf("sc2", [128, 256], F32)
        selb = S.sbuf("selb", [128, 256], BF16)
        m8 = S.sbuf("m8", [128, 16], F32)
        selT = S.sbuf("selT", [128, 2, 512], BF16)
        mselT = S.sbuf("mselT", [64, 2, 512], BF16)
        msc = S.sbuf("msc", [128, NB], F32)
        mselb = S.sbuf("mselb", [128, NB], BF16)
        rr = S.sbuf("rr", [128, 8], F32)
        oacc = S.sbuf("oacc", [128, 4, 128], F32)
        macc = S.sbuf("macc", [128, 4, 128], F32)
        oaccb = S.sbuf("oaccb", [128, 4, 128], BF16)
        maccb = S.sbuf("maccb", [128, 4, 128], BF16)
        oTs = S.sbuf("oTs", [65, 512], F32)
        kbuf = [S.sbuf("kbuf%d" % i, [64, 512], BF16) for i in range(2)]
        vbuf = [S.sbuf("vbuf%d" % i, [128, 4, 66], BF16) for i in range(2)]
        kwbuf = [S.sbuf("kwbuf%d" % i, [64, 512], BF16) for i in range(2)]
        vwbuf = [S.sbuf("vwbuf%d" % i, [128, 4, 66], BF16) for i in range(2)]
        mkbuf = [S.sbuf("mkbuf%d" % i, [64, 2, 512], BF16) for i in range(2)]
        mvbuf = [S.sbuf("mvbuf%d" % i, [128, 4, 2, 66], BF16) for i in range(2)]
        for i in range(2):
            S.memset(vbuf[i][:], 1.0, [vbuf[i]])
            S.memset(vwbuf[i][:], 1.0, [vwbuf[i]])
            S.memset(mvbuf[i][:], 1.0, [mvbuf[i]])
        cnt = {"e": 0, "m": 0, "o": 0, "ld": 0}

        def attn_unit(qT_ap, qbuf, kT_ap, kbuf_, vx_ap, vbuf_, mT_lhs, mT_rhs, mbufs, out_ps, first, last,
                      sel1=None, sel2=None):
            i = cnt["e"] % 4
            cnt["e"] += 1
            sps = PB[i]
            if mT_lhs is not None:
                S.mm(sps[:], kT_ap, qT_ap, True, False, [kbuf_, qbuf], [sps])
                S.mm(sps[:], mT_lhs, mT_rhs, False, True, mbufs, [sps])
            else:
                S.mm(sps[:], kT_ap, qT_ap, True, True, [kbuf_, qbuf], [sps])
            S.act(Eb[i][:], sps[:], AF.Exp, [sps], [Eb[i]], scale=SCALE)
            src = Eb[i]
            for (base, cm, step) in (sel1, sel2):
                if base is None:
                    continue
                S.op("pool", lambda e, src=src, base=base, cm=cm, step=step: e.affine_select(
                    out=src[:], in_=src[:], pattern=[[step, 512]], compare_op=ALU.is_ge, fill=0.0,
                    base=base, channel_multiplier=cm), [src], [src])
            pending.append((out_ps, vx_ap, src, first, last, vbuf_))
            flush(PIPE)

        pending = []
        PIPE = 2

        def flush(keep):
            while len(pending) > keep:
                out_ps, vx_ap, src, first, last, vbuf_ = pending.pop(0)
                S.mm(out_ps[0:65, :], vx_ap, src[:], first, last, [vbuf_, src], [out_ps])

        NOSEL = (None, None, None)
        OWNJ = [(0, 0, 0), (1, 64, 3)]

        def finalize(out_ps, acc, col0, gate_col, first_branch):
            flush(0)
            S.copy(oTs[:], out_ps[0:65, :], [out_ps], [oTs], eng="act")
            fp = PB[6]
            for tq in range(4):
                S.tr(fp[:, tq * 65:(tq + 1) * 65], oTs[:, tq * 128:(tq + 1) * 128], identf[0:65, 0:65], [oTs, identf], [fp])
            fv = fp[:, 0:260].rearrange("p (t c) -> p t c", c=65)
            S.ts(rr[:, 0:4], fv[:, :, 64], 1e-30, None, ALU.max, None, [fp], [rr])
            S.op("dve", lambda e: e.reciprocal(out=rr[:, 4:8], in_=rr[:, 0:4]), [rr], [rr])
            if gate_col is not None:
                S.tt(rr[:, 4:8], rr[:, 4:8], gts[:, :, gate_col], ALU.mult, [rr, gts], [rr])
            for tq in range(4):
                dst = acc[:, tq, col0:col0 + 64]
                if first_branch:
                    S.ts(dst, fv[:, tq, 0:64], rr[:, 4 + tq:5 + tq], None, ALU.mult, None, [fp, rr], [acc])
                else:
                    S.stt(dst, fv[:, tq, 0:64], rr[:, 4 + tq:5 + tq], dst, ALU.mult, ALU.add, [fp, rr, acc], [acc])

        for qc in range(NQC):
            q0 = qc * 512
            S.dma(qraw[:, 0:2, :].bitcast(F32), qrawA.t[:, :, q0 // 2:q0 // 2 + 256].rearrange("h d t -> d h t"), reads=[qrawA], writes=[qraw])
            S.dma(qraw[:, 2:4, :].bitcast(F32), qrawB.t[:, :, q0 // 2:q0 // 2 + 256].rearrange("h d t -> d h t"), reads=[qrawB], writes=[qraw])
            S.dma(qrot[:].bitcast(F32), qrot_d.t[:, :, q0 // 2:q0 // 2 + 256].rearrange("h d t -> d h t"), reads=[qrot_d], writes=[qrot])
            S.dma(mq[:].bitcast(F32), mq_d.t[:, :, q0 // 2:q0 // 2 + 256].rearrange("h d t -> d h t"), reads=[mq_d], writes=[mq])
            S.dma(gts[:], gates_d.t[q0:q0 + 512, :].rearrange("(t p) c -> p t c", p=128), reads=[gates_d], writes=[gts])
            tmax = q0 + 511
            cmax = (tmax - 31) // 16 if tmax >= 31 else -1
            cmax = min(cmax, NCMP - 1)
            nct = cmax // 128 + 1 if cmax >= 0 else 0
            S.memset(imp[:], 0.0, [imp], eng="dve")
            for j in range(4):
                for ct in range(nct):
                    i = cnt["e"] % 2
                    cnt["e"] += 1
                    sps = PB[i]
                    S.mm(sps[:], kcT[:, ct * 128:(ct + 1) * 128], qraw[:, j, :], True, True, [kcT, qraw], [sps])
                    S.act(Eb[i][:], sps[:], AF.Exp, [sps], [Eb[i]], scale=SCALE)
                    base = q0 - 2048 * ct - 31
                    if base - 16 * 127 < 0:
                        S.op("pool", lambda e, src=Eb[i], base=base: e.affine_select(
                            out=src[:], in_=src[:], pattern=[[1, 512]], compare_op=ALU.is_ge, fill=0.0,
                            base=base, channel_multiplier=-16), [Eb[i]], [Eb[i]])
                    for tq in range(4):
                        S.mm(PB[2 + tq][:, 0:321], Eb[i][:, tq * 128:(tq + 1) * 128], vcx[:, ct, :],
                             ct == 0, ct == nct - 1, [Eb[i], vcx], [PB[2 + tq]])
                if nct == 0:
                    continue
                for tq in range(4):
                    ps = PB[2 + tq]
                    S.ts(rr[:, 0:1], ps[:, 320:321], 1e-30, None, ALU.max, None, [ps], [rr])
                    S.op("dve", lambda e: e.reciprocal(out=rr[:, 1:2], in_=rr[:, 0:1]), [rr], [rr])
                    S.stt(imp[:, tq, :], ps[:, 0:256], rr[:, 1:2], imp[:, tq, :], ALU.mult, ALU.add, [ps, rr, imp], [imp])
                    for (jj, col0, gcol) in OWNJ:
                        if jj == j:
                            S.tt(rr[:, 2:3], rr[:, 1:2], gts[:, tq, gcol:gcol + 1], ALU.mult, [rr, gts], [rr])
                            S.ts(oacc[:, tq, col0:col0 + 64], ps[:, 256:320], rr[:, 2:3], None, ALU.mult, None,
                                 [ps, rr], [oacc])
            if nct == 0:
                S.memset(oacc[:], 0.0, [oacc], eng="dve")
            for tq in range(4):
                T = qc * 4 + tq
                S.copy(sc[:], imp[:, tq, :], [imp], [sc])
                S.memset(sc[:, 0:1], 1e4, [sc], eng="dve")
                lo = max(2 * T - 1, 0)
                S.memset(sc[0:64, lo:2 * T + 1], 1e4, [sc], eng="dve")
                S.memset(sc[64:128, 2 * T:2 * T + 2], 1e4, [sc], eng="dve")
                if 2 * T + 1 < 256:
                    S.memset(sc[0:64, 2 * T + 1:256], NEG, [sc], eng="dve")
                if 2 * T + 2 < 256:
                    S.memset(sc[64:128, 2 * T + 2:256], NEG, [sc], eng="dve")
                S.op("dve", lambda e: e.max(out=m8[:, 0:8], in_=sc[:]), [sc], [m8])
                S.op("dve", lambda e: e.match_replace(out=sc2[:], in_to_replace=m8[:, 0:8], in_values=sc[:],
                                                      imm_value=-3.0e38), [sc, m8], [sc2])
                S.op("dve", lambda e: e.max(out=m8[:, 8:16], in_=sc2[:]), [sc2], [m8])
                S.ts(selb[:], sc[:], m8[:, 15:16], None, ALU.is_ge, None, [sc, m8], [selb])
                if 2 * T + 1 < 256:
                    S.memset(selb[0:64, 2 * T + 1:256], 0.0, [selb], eng="dve")
                if 2 * T + 2 < 256:
                    S.memset(selb[64:128, 2 * T + 2:256], 0.0, [selb], eng="dve")
                tp = PB[7]
                tpv = tp[:].bitcast(BF16)
                for bt in range(2):
                    S.tr(tpv[:, bt * 128:(bt + 1) * 128], selb[:, bt * 128:(bt + 1) * 128], identb[:], [selb, identb], [tp])
                S.act(selT[:, :, tq * 128:(tq + 1) * 128], tpv[:, 0:256].rearrange("p (b q) -> p b q", q=128),
                      AF.Identity, [tp], [selT], scale=MASKBIG, bias=-MASKBIG)
            for h in range(2):
                for tq in range(4):
                    cur = (q0 + tq * 128) // 256
                    gp = PB[7]
                    S.mm(gp[:, 0:NB], mq[:, h, tq * 128:(tq + 1) * 128], kmb[:, h, :], True, True, [mq, kmb], [gp])
                    S.copy(msc[:], gp[:, 0:NB], [gp], [msc])
                    S.memset(msc[:, cur:NB], NEG, [msc], eng="dve")
                    S.op("dve", lambda e: e.max(out=m8[:, 0:8], in_=msc[:]), [msc], [m8])
                    S.ts(mselb[:], msc[:], m8[:, 2:3], None, ALU.is_ge, None, [msc, m8], [mselb])
                    S.memset(mselb[:, cur:NB], 0.0, [mselb], eng="dve")
                    S.memset(mselb[:, cur:cur + 1], 1.0, [mselb], eng="dve")
                    tpv = gp[:].bitcast(BF16)
                    S.tr(tpv[0:NB, 512:640], mselb[:, :], identb[:], [mselb, identb], [gp])
                    S.act(mselT[0:NB, h, tq * 128:(tq + 1) * 128], tpv[0:NB, 512:640], AF.Identity, [gp], [mselT],
                          scale=MASKBIG, bias=-MASKBIG)
            outS = [PB[4], PB[5]]
            nkg = qc + 1
            mo_out = [None, None]
            for kg in range(nkg):
                li = cnt["ld"] % 2
                cnt["ld"] += 1
                k0 = kg * 512
                S.dma(kbuf[li][:].bitcast(F32), ks_d.t[:, k0 // 2:k0 // 2 + 256], reads=[ks_d], writes=[kbuf[li]])
                S.dma(vbuf[li][:, :, 0:64].bitcast(F32),
                      vs_d.t[k0:k0 + 512, :].rearrange("(t p) c -> p t c", p=128), reads=[vs_d], writes=[vbuf[li]])
                for h in range(2):
                    for t4 in range(4):
                        kt = kg * 4 + t4
                        diag = kt >= 4 * qc
                        attn_unit(qrot[:, h, :], qrot, kbuf[li][:, t4 * 128:(t4 + 1) * 128], kbuf[li],
                                  vbuf[li][:, t4, 0:65], vbuf[li],
                                  Ex[:, kt % 64, :], selT[:, kt // 64, :], [Ex, selT],
                                  outS[h], kt == 0, kt == 4 * qc + 3,
                                  sel1=(q0 - 128 * kt, -1, 1) if diag else NOSEL, sel2=NOSEL)
            for h in range(2):
                finalize(outS[h], oacc, h * 64, 3 * h + 1, False)
            for h in range(2):
                wout = PB[4 + h]
                kts = list(range(max(0, 4 * qc - 4), 4 * qc + 4))
                for kg in sorted(set(k // 4 for k in kts)):
                    li = cnt["ld"] % 2
                    cnt["ld"] += 1
                    k0 = kg * 512
                    S.dma(kwbuf[li][:].bitcast(F32), kw_d.t[:, k0 // 2:k0 // 2 + 256], reads=[kw_d], writes=[kwbuf[li]])
                    S.dma(vwbuf[li][:, :, 0:64].bitcast(F32),
                          vw_d.t[k0:k0 + 512, :].rearrange("(t p) c -> p t c", p=128), reads=[vw_d], writes=[vwbuf[li]])
                    for t4 in range(4):
                        kt = kg * 4 + t4
                        diag = kt >= 4 * qc
                        attn_unit(qrot[:, h, :], qrot, kwbuf[li][:, t4 * 128:(t4 + 1) * 128], kwbuf[li],
                                  vwbuf[li][:, t4, 0:65], vwbuf[li], None, None, None,
                                  wout, kt == kts[0], kt == kts[-1],
                                  sel1=(q0 - 128 * kt, -1, 1) if diag else NOSEL,
                                  sel2=(128 * kt - q0 + 511, 1, -1) if not diag else NOSEL)
                finalize(wout, oacc, h * 64, 3 * h + 2, False)
            for h in range(2):
                mout = PB[4 + h]
                for kg in range(nkg):
                    li = cnt["ld"] % 2
                    cnt["ld"] += 1
                    k0 = kg * 512
                    S.dma(mkbuf[li][:, 0, :].bitcast(F32), mk_d.t[h, :, k0 // 2:k0 // 2 + 256], reads=[mk_d], writes=[mkbuf[li]])
                    S.dma(mvbuf[li][:, :, 0, 0:64].bitcast(F32),
                          mv_d.t[k0:k0 + 512, 32 * h:32 * h + 32].rearrange("(t p) c -> p t c", p=128),
                          reads=[mv_d], writes=[mvbuf[li]])
                    for t4 in range(4):
                        kt = kg * 4 + t4
                        diag = kt >= 4 * qc
                        attn_unit(mq[:, h, :], mq, mkbuf[li][:, 0, t4 * 128:(t4 + 1) * 128], mkbuf[li],
                                  mvbuf[li][:, t4, 0, 0:65], mvbuf[li],
                                  Rm[0:NB, kt // 2, :], mselT[0:NB, h, :], [Rm, mselT],
                                  mout, kt == 0, kt == 4 * qc + 3,
                                  sel1=(q0 - 128 * kt, -1, 1) if diag else NOSEL, sel2=NOSEL)
                finalize(mout, macc, h * 64, None, True)
            S.copy(oaccb[:], oacc[:], [oacc], [oaccb])
            S.copy(maccb[:], macc[:], [macc], [maccb])
            S.dma(onsa_d.t[q0:q0 + 512, :].rearrange("(t p) c -> p t c", p=128), oaccb[:].bitcast(F32),
                  reads=[oaccb], writes=[onsa_d])
            S.dma(omoba_d.t[q0:q0 + 512, :].rearrange("(t p) c -> p t c", p=128), maccb[:].bitcast(F32),
                  reads=[maccb], writes=[omoba_d])
        S.emit_block(st)
        S.stack = S.outer
    return nc


def _cat(resA, b, key):
    return [np.asarray(resA[4 * b + p][key]) for p in range(4)]


def host_inputs_C(resA, inp, l, S):
    maps = []
    peT = _f32(np.transpose(inp["cmp_pe"][l], (0, 2, 1)))
    w1 = _f32(inp["cmp_w1"][l])
    w2 = _f32(inp["cmp_w2"][l])
    for b in range(2):
        oT = np.concatenate(_cat(resA, b, "oT"), axis=2)
        oQ = np.concatenate(_cat(resA, b, "oQ"), axis=2)
        oM = np.concatenate(_cat(resA, b, "oM"), axis=0)
        oG = np.concatenate(_cat(resA, b, "oG"), axis=0)
        oK = np.concatenate(_cat(resA, b, "oK"), axis=2)
        for g in range(4):
            kv = g // 2
            own = [2 * g, 2 * g + 1]
            oth = [h for h in range(4 * kv, 4 * kv + 4) if h not in own]
            maps.append({
                "qraw": _f32(oQ[own + oth]),
                "qrot": _f32(oT[own]),
                "kcp": _f32(np.stack([oT[8 + kv], oT[10 + kv]])),
                "ksT": _f32(oT[12 + kv]), "kwT": _f32(oT[14 + kv]),
                "vsw": _f32(np.concatenate([oM[:, 32 * kv:32 * kv + 32], oM[:, 64 + 32 * kv:64 + 32 * kv + 32]], axis=1)),
                "gates": _f32(oG[:, 6 * g:6 * g + 6]),
                "mq": _f32(oT[[16 + 2 * g, 17 + 2 * g]]), "mk": _f32(oT[[24 + 2 * g, 25 + 2 * g]]),
                "mv": _f32(oM[:, 128 + 64 * g:128 + 64 * g + 64]),
                "kmean": _f32(oK[own]),
                "peT": peT, "w1": w1, "w2": w2,
            })
    return maps


def build_stageD1(TL, S=None, io=None, pfx=""):
    alone = S is None
    nc = bass.Bass("TRN2", target_bir_lowering=False) if alone else S.nc
    NT = TL // 128
    with ExitStack() as st:
        if alone:
            S = Sched(nc, st)
        S.stack = st
        S.pfx = pfx
        _in = (lambda n, sh, dt: S.din(n, sh, dt)) if alone else (lambda n, sh, dt: io[n])
        _out = (lambda n, sh, dt: S.dout(n, sh, dt)) if alone else (lambda n, sh, dt: io[n])
        xs = _in("xs", [TL, D], F32)
        tokmaj = (not alone) and ("oN" in io)
        if tokmaj:
            oN_d, oMo_d = io["oN"], io["oMo"]
        else:
            onT_d = _in("onT", [512, TL // 2], F32)
            omT_d = _in("omT", [512, TL // 2], F32)
        cT_d = _in("cT", [128, 8], F32)
        wada = _in("wada", [D, 3072], F32)
        bada = _in("bada", [128, 3072], F32)
        gat = _in("gattn", [128, D], F32)
        wG_d = _in("wG", [D, 2048], F32)
        wun_d = _in("wupn", [512, D], F32)
        wum_d = _in("wupm", [512, D], F32)
        wo_d = _in("wout", [D, D], F32)
        xo = _out("xo", [TL, D], F32)

        identb = make_ident(S, BF16, "identb")
        PB = [S.psum("PB%d" % i, [128, 512], F32) for i in range(8)]
        stage = S.sbuf("wstage", [128, 8, 512], F32)
        wG = S.sbuf("wGb", [128, 8, 2048], BF16)
        wun = S.sbuf("wunb", [128, 4, D], BF16)
        wum = S.sbuf("wumb", [128, 4, D], BF16)
        wo = S.sbuf("wob", [128, 8, D], BF16)
        load_cast_weight(S, wG, 0, wG_d, 2048, stage)
        load_cast_weight(S, wo, 0, wo_d, D, stage)
        for (dst, src) in ((wun, wun_d), (wum, wum_d)):
            for c0 in (0, 512):
                S.dma(stage[:, 0:4, :], src.t[:, c0:c0 + 512].rearrange("(k p) n -> p k n", p=128), writes=[stage])
                S.copy(dst[:, :, c0:c0 + 512], stage[:, 0:4, :], [stage], [dst], eng="pool")
        mod = compute_mod(S, cT_d, wada, bada, 3072, "modD", stage, ps=PB[0])
        gt = S.sbuf("gat", [128, D], F32)
        S.dma(gt[:], gat.t[:, :], writes=[gt])
        A1 = S.sbuf("A1", [128, D], F32)
        S.stt(A1[:], mod[:, 1024:2048], 1.0, gt[:], ALU.add, ALU.mult, [mod, gt], [A1])
        B1 = mod[:, 0:1024]
        GA1 = mod[:, 2048:3072]

        xt = [S.sbuf("xt%d" % i, [128, D], F32) for i in range(2)]
        scr = S.sbuf("scr", [128, D], F32)
        small = S.sbuf("small", [128, 4], F32)
        hb = S.sbuf("hb", [128, D], BF16)
        hT = S.sbuf("hT", [128, 8, 128], BF16)
        onT = S.sbuf("onTs", [128, 4, 128], BF16)
        omT = S.sbuf("omTs", [128, 4, 128], BF16)
        otm = S.sbuf("otm", [128, 2, 512], BF16)
        sg = S.sbuf("sg", [128, 16, 128], F32)
        y1 = S.sbuf("y1", [128, 8, 128], F32)
        y2 = S.sbuf("y2", [128, 8, 128], F32)
        yT = S.sbuf("yT", [128, 8, 128], BF16)
        xo_t = S.sbuf("xo_t", [128, D], F32)
        pstr = PB[7]
        for it in range(NT):
            t0 = it * 128
            xb = xt[it % 2]
            S.dma(xb[:], xs.t[t0:t0 + 128, :], reads=[xs], writes=[xb])
            if tokmaj:
                S.dma(otm[:, 0, :].bitcast(F32), oN_d.t[t0:t0 + 128, :], reads=[oN_d], writes=[otm])
                S.dma(otm[:, 1, :].bitcast(F32), oMo_d.t[t0:t0 + 128, :], reads=[oMo_d], writes=[otm])
                for j_, dst_ in ((0, onT), (1, omT)):
                    pv2 = PB[6][:].bitcast(BF16)
                    for k in range(4):
                        S.tr(pv2[:, k * 128:(k + 1) * 128], otm[:, j_, k * 128:(k + 1) * 128], identb[:], [otm, identb], [PB[6]])
                    S.copy(dst_[:], pv2[:, 0:512].rearrange("p (k t) -> p k t", t=128), [PB[6]], [dst_], eng="act")
            else:
                S.dma(onT[:].bitcast(F32), onT_d.t[:, t0 // 2:t0 // 2 + 64].rearrange("(k p) t -> p k t", p=128), writes=[onT])
                S.dma(omT[:].bitcast(F32), omT_d.t[:, t0 // 2:t0 // 2 + 64].rearrange("(k p) t -> p k t", p=128), writes=[omT])
            rmsnorm_mod_tile(S, xb[:], xb, A1, B1, hb[:], hb, scr, small, Bbuf=mod)
            pv = pstr[:].bitcast(BF16)
            for k in range(8):
                S.tr(pv[:, k * 128:(k + 1) * 128], hb[:, k * 128:(k + 1) * 128], identb[:], [hb, identb], [pstr])
            S.copy(hT[:], pv[:].rearrange("p (k t) -> p k t", t=128), [pstr], [hT], eng="act")
            for c in range(16):
                pb = PB[c // 4]
                for k in range(8):
                    S.mm(pb[:, (c % 4) * 128:(c % 4 + 1) * 128], wG[:, k, c * 128:(c + 1) * 128], hT[:, k, :],
                         k == 0, k == 7, [wG, hT], [pb])
                if c % 4 == 3:
                    S.act(sg[:, c - 3:c + 1, :], pb[:].rearrange("p (c t) -> p c t", t=128), AF.Sigmoid, [pb], [sg])
            for c in range(16):
                pb = PB[4 + (c // 4) % 2] if False else PB[c // 4]
                w = wun if c < 8 else wum
                o = onT if c < 8 else omT
                cc = c % 8
                for kk in range(4):
                    S.mm(pb[:, (c % 4) * 128:(c % 4 + 1) * 128], w[:, kk, cc * 128:(cc + 1) * 128], o[:, kk, :],
                         kk == 0, kk == 3, [w, o], [pb])
                if c % 4 == 3:
                    dst = y1 if c < 8 else y2
                    c4 = (c % 8) - 3
                    S.tt(dst[:, c4:c4 + 4, :], pb[:].rearrange("p (c t) -> p c t", t=128), sg[:, c - 3:c + 1, :],
                         ALU.mult, [pb, sg], [dst])
            S.tt(yT[:], y1[:], y2[:], ALU.add, [y1, y2], [yT])
            for half in range(2):
                pb = PB[4 + half]
                for c in range(8):
                    S.mm(pb[:], yT[:, c, :], wo[:, c, half * 512:(half + 1) * 512], c == 0, c == 7, [yT, wo], [pb])
                S.tt(scr[:, half * 512:(half + 1) * 512], pb[:], GA1[:, half * 512:(half + 1) * 512], ALU.mult,
                     [pb, mod], [scr])
            S.tt(xo_t[:], scr[:], xb[:], ALU.add, [scr, xb], [xo_t])
            S.dma(xo.t[t0:t0 + 128, :], xo_t[:], reads=[xo_t], writes=[xo])
        S.emit_block(st)
        S.stack = S.outer
    return nc


def host_inputs_D1(resC, inp, l, x_cur, S):
    TL = S // 4
    w_in = inp["w_in"][l]
    wG = _f32(w_in[:, C_GN:C_GN + 2048])
    wada = _f32(inp["w_ada"][l][:, 0:3072])
    bada = _f32(np.broadcast_to(inp["b_ada"][l][None, 0:3072], (128, 3072)))
    gat = _f32(np.broadcast_to(inp["g_attn"][l][None, :], (128, D)))
    maps = []
    for b in range(2):
        on = np.concatenate([np.ascontiguousarray(np.asarray(resC[4 * b + g]["onsa"])).view(ml_dtypes.bfloat16)
                             for g in range(4)], axis=1)
        om = np.concatenate([np.ascontiguousarray(np.asarray(resC[4 * b + g]["omoba"])).view(ml_dtypes.bfloat16)
                             for g in range(4)], axis=1)
        for g in range(4):
            sl = slice(g * TL, (g + 1) * TL)
            onT = np.ascontiguousarray(on[sl].T).view(np.float32)
            omT = np.ascontiguousarray(om[sl].T).view(np.float32)
            maps.append({
                "xs": _f32(x_cur[b, sl]), "onT": onT, "omT": omT,
                "cT": _f32(inp["c"][b].reshape(8, 128).T),
                "wada": wada, "bada": bada, "gattn": gat, "wG": wG,
                "wupn": _f32(inp["w_up_nsa"][l]), "wupm": _f32(inp["w_up_moba"][l]), "wout": _f32(inp["w_out"][l]),
            })
    return maps


def build_stageD2(TL, last, S=None, io=None, pfx=""):
    alone = S is None
    nc = bass.Bass("TRN2", target_bir_lowering=False) if alone else S.nc
    NT = TL // 128
    NE = 16384
    with ExitStack() as st:
        if alone:
            S = Sched(nc, st)
        S.stack = st
        S.pfx = pfx
        _in = (lambda n, sh, dt: S.din(n, sh, dt)) if alone else (lambda n, sh, dt: io[n])
        _out = (lambda n, sh, dt: S.dout(n, sh, dt)) if alone else (lambda n, sh, dt: io[n])
        xs = _in("xs", [TL, D], F32)
        cT_d = _in("cT", [128, 8], F32)
        wada = _in("wada", [D, 3072], F32)
        bada = _in("bada", [128, 3072], F32)
        gff = _in("gffn", [128, D], F32)
        gfin = _in("gfinal", [128, D], F32)
        wq_d = _in("wq", [D, 2048], F32)
        kT_d = _in("kT", [2, 128, 128], F32)
        UT_d = _in("UT", [D, NE], F32)
        V_d = _in("V", [NE, D], F32)
        xo = _out("xo", [TL, D], F32)
        UTb = S.dscr("UTb" + pfx, [128, 8, NE], BF16)
        Vb = S.dscr("Vb" + pfx, [128, 128, D], BF16)

        identb = make_ident(S, BF16, "identb")
        PB = [S.psum("PB%d" % i, [128, 512], F32) for i in range(8)]
        utb = [S.sbuf("utb%d" % i, [128, 8, 1024], BF16) for i in range(2)]
        vb = [S.sbuf("vb%d" % i, [128, 8, 1024], BF16) for i in range(2)]
        gall = S.sbuf("gall", [128, 8, 8, 128], BF16)
        cst = [gall.view("cst%d" % i, gall[:, 4 * i:4 * i + 4, :, :].rearrange("p a b c -> p (a b c)").rearrange(
            "p (k n) -> p k n", n=512)) for i in range(2)]
        engs = ["pool", "dve", "act"]
        r = 0
        for e0 in range(0, NE, 512):
            stg = utb[r % 2]
            sv = stg[:].bitcast(F32)
            S.dma(sv, UT_d.t[:, e0:e0 + 512].rearrange("(k p) n -> p k n", p=128), writes=[stg])
            S.copy(cst[r % 2][:], sv, [stg], [cst[r % 2]], eng=engs[r % 3])
            S.dma(UTb.t[:, :, e0:e0 + 512], cst[r % 2][:], reads=[cst[r % 2]], writes=[UTb])
            r += 1
        for t0 in range(0, 128, 4):
            stg = vb[r % 2]
            sv = stg[:].bitcast(F32).rearrange("p k (a n) -> p (k a) n", a=1) if False else stg[:].bitcast(F32)
            sv4 = sv.rearrange("p (t h) n -> p t (h n)", h=2)
            S.dma(sv4, V_d.t[t0 * 128:(t0 + 4) * 128, :].rearrange("(t p) d -> p t d", p=128), writes=[stg])
            cv = cst[r % 2][:].rearrange("p (t h) n -> p t (h n)", h=2)
            S.copy(cv, sv4, [stg], [cst[r % 2]], eng=engs[r % 3])
            S.dma(Vb.t[:, t0:t0 + 4, :], cv, reads=[cst[r % 2]], writes=[Vb])
            r += 1
        wq = S.sbuf("wqb", [128, 8, 2048], BF16)
        stage = vb[0].view("wstage", vb[0][:].bitcast(F32))
        load_cast_weight(S, wq, 0, wq_d, 2048, stage)
        kst = S.sbuf("kst", [128, 2, 128], F32)
        kTb = S.sbuf("kTb", [128, 2, 128], BF16)
        S.dma(kst[:], kT_d.t[:, :, :].rearrange("j d n -> d j n"), writes=[kst])
        S.copy(kTb[:], kst[:], [kst], [kTb])
        mod = compute_mod(S, cT_d, wada, bada, 3072, "modE", stage, ps=PB[0])
        gel = S.sbuf("gel", [128, 3, 1024], F32)
        gt = gel.view("gff", gel[:, 0, :])
        S.dma(gt[:], gff.t[:, :], writes=[gt])
        A2 = S.sbuf("A2", [128, D], F32)
        S.stt(A2[:], mod[:, 1024:2048], 1.0, gt[:], ALU.add, ALU.mult, [mod, gt], [A2])
        B2 = mod[:, 0:1024]
        GA2 = mod[:, 2048:3072]
        gfn = S.sbuf("gfn", [128, D], F32)
        if last:
            S.dma(gfn[:], gfin.t[:, :], writes=[gfn])

        xt = [S.sbuf("xt%d" % i, [128, D], F32) for i in range(2)]
        scr = S.sbuf("scr", [128, D], F32)
        small = S.sbuf("small", [128, 4], F32)
        hb = S.sbuf("hb", [128, D], BF16)
        hT = S.sbuf("hT", [128, 8, 128], BF16)
        qT = gall.view("qT", gall[:, 0:2, :, :].rearrange("p a b c -> p (a b) c"))
        sc = S.sbuf("sc", [128, 16, 128], F32)
        m16 = S.sbuf("m16", [128, 16, 16], F32)
        tmpa = S.sbuf("tmpa", [128, 256], F32)
        c16 = S.sbuf("c16", [128, 8, 16], F32)
        e16 = S.sbuf("e16", [128, 16], F32)
        st8 = S.sbuf("st8", [128, 4, 8], F32)
        xg = [S.sbuf("xg%d" % i, [128, 8, 128], F32) for i in range(2)]
        exb = [S.sbuf("exb%d" % i, [128, 8, 128], BF16) for i in range(2)]
        gab = [S.sbuf("gab%d" % i, [128, 8, 128], BF16) for i in range(2)]
        cnt = {"x": 0, "ld": 0}
        hTs = [hT, S.sbuf("hT1", [128, 8, 128], BF16)]
        scs = [sc, S.sbuf("sc1", [128, 16, 128], F32)]
        c16s = [c16, S.sbuf("c161", [128, 8, 16], F32)]
        st8s = [st8, S.sbuf("st81", [128, 4, 8], F32)]
        assert NT % 2 == 0
        for ip in range(NT // 2):
            for u in range(2):
                it = 2 * ip + u
                t0 = it * 128
                xb = xt[u]
                hT_, sc_, c16_, st8_ = hTs[u], scs[u], c16s[u], st8s[u]
                S.dma(xb[:], xs.t[t0:t0 + 128, :], reads=[xs], writes=[xb])
                rmsnorm_mod_tile(S, xb[:], xb, A2, B2, hb[:], hb, scr, small, Bbuf=mod)
                pstr = PB[4]
                pv = pstr[:].bitcast(BF16)
                for k in range(8):
                    S.tr(pv[:, k * 128:(k + 1) * 128], hb[:, k * 128:(k + 1) * 128], identb[:], [hb, identb], [pstr])
                S.copy(hT_[:], pv[:].rearrange("p (k t) -> p k t", t=128), [pstr], [hT_], eng="act")
                for c in range(16):
                    pb = PB[c // 4]
                    for k in range(8):
                        S.mm(pb[:, (c % 4) * 128:(c % 4 + 1) * 128], wq[:, k, c * 128:(c + 1) * 128], hT_[:, k, :],
                             k == 0, k == 7, [wq, hT_], [pb])
                    if c % 4 == 3:
                        S.copy(qT[:, c - 3:c + 1, :], pb[:].rearrange("p (c t) -> p c t", t=128), [pb], [qT], eng="act")
                for c in range(16):
                    pb = PB[c // 4]
                    S.mm(pb[:, (c % 4) * 128:(c % 4 + 1) * 128], qT[:, c, :], kTb[:, c % 2, :], True, True, [qT, kTb], [pb])
                    if c % 4 == 3:
                        S.copy(sc_[:, c - 3:c + 1, :], pb[:].rearrange("p (c t) -> p c t", t=128), [pb], [sc_], eng="act")
                for c in range(16):
                    S.op("dve", lambda e, c=c, sc_=sc_: e.max(out=m16[:, c, 0:8], in_=sc_[:, c, :]), [sc_], [m16])
                    S.op("dve", lambda e, c=c, sc_=sc_: e.match_replace(out=tmpa[:, 0:128], in_to_replace=m16[:, c, 0:8],
                                                                        in_values=sc_[:, c, :], imm_value=-3.0e38), [sc_, m16], [tmpa])
                    S.op("dve", lambda e, c=c: e.max(out=m16[:, c, 8:16], in_=tmpa[:, 0:128]), [tmpa], [m16])
                cand = gel[:, 0:2, :].rearrange("p a (h x) -> p (a h) x", x=256)
                m4 = m16[:].rearrange("p (h two) k -> p h two k", two=2)
                S.tt(cand.rearrange("p h (a b) -> p h a b", b=16),
                     m4[:, :, 0, :].unsqueeze(3).to_broadcast([128, 8, 16, 16]),
                     m4[:, :, 1, :].unsqueeze(2).to_broadcast([128, 8, 16, 16]), ALU.add, [m16], [gel])
                for h in range(8):
                    S.op("dve", lambda e, h=h, c16_=c16_: e.max(out=c16_[:, h, 0:8], in_=cand[:, h, :]), [gel], [c16_])
                    S.op("dve", lambda e, h=h, c16_=c16_: e.match_replace(out=tmpa[:], in_to_replace=c16_[:, h, 0:8],
                                                                          in_values=cand[:, h, :], imm_value=-3.0e38), [gel, c16_], [tmpa])
                    S.op("dve", lambda e, h=h, c16_=c16_: e.max(out=c16_[:, h, 8:16], in_=tmpa[:]), [tmpa], [c16_])
                S.ts(st8_[:, 0, :], c16_[:, :, 0], -1.0, None, ALU.mult, None, [c16_], [st8_])
                for h in range(8):
                    S.act(e16[:], c16_[:, h, :], AF.Exp, [c16_, st8_], [e16, st8_], bias=st8_[:, 0, h:h + 1],
                          accum_out=st8_[:, 1, h:h + 1])
                S.act(st8_[:, 2, :], st8_[:, 1, :], AF.Ln, [st8_], [st8_])
                S.tt(st8_[:, 3, :], st8_[:, 0, :], st8_[:, 2, :], ALU.subtract, [st8_], [st8_])
            for ec in range(16):
                li = cnt["ld"] % 2
                cnt["ld"] += 1
                S.dma(utb[li][:], UTb.t[:, :, ec * 1024:(ec + 1) * 1024], reads=[UTb], writes=[utb[li]])
                S.dma(vb[li][:], Vb.t[:, ec * 8:(ec + 1) * 8, :], reads=[Vb], writes=[vb[li]])
                for u in range(2):
                    hT_, sc_, c16_, st8_ = hTs[u], scs[u], c16s[u], st8s[u]
                    for h in range(8):
                        xi = cnt["x"] % 2
                        cnt["x"] += 1
                        S.tt(xg[xi][:], sc_[:, 2 * h, ec * 8:(ec + 1) * 8].unsqueeze(2).to_broadcast([128, 8, 128]),
                             sc_[:, 2 * h + 1, :].unsqueeze(1).to_broadcast([128, 8, 128]), ALU.add, [sc_], [xg[xi]])
                        S.act(exb[xi][:], xg[xi][:], AF.Exp, [xg[xi], st8_], [exb[xi]], bias=st8_[:, 3, h:h + 1])
                        S.stt(gall[:, h, :, :], xg[xi][:], c16_[:, h, 15:16], exb[xi][:], ALU.is_ge, ALU.mult,
                              [xg[xi], c16_, exb[xi]], [gall])
                    for ii in range(8):
                        pb = PB[ii // 4]
                        for h in range(8):
                            S.mm(pb[:, (ii % 4) * 128:(ii % 4 + 1) * 128], gall[:, h, ii, :], identb[:], h == 0, h == 7,
                                 [gall, identb], [pb])
                    for ii in range(8):
                        pb = PB[2 + ii // 4]
                        for k in range(8):
                            S.mm(pb[:, (ii % 4) * 128:(ii % 4 + 1) * 128], utb[li][:, k, ii * 128:(ii + 1) * 128], hT_[:, k, :],
                                 k == 0, k == 7, [utb[li], hT_], [pb])
                    S.copy(gel[:, 0, 0:512], PB[2][:], [PB[2]], [gel], eng="act")
                    S.copy(gel[:, 0, 512:1024], PB[3][:], [PB[3]], [gel], eng="act")
                    S.act(gel[:, 1, :], gel[:, 0, :], AF.Square, [gel], [gel])
                    S.ts(gel[:, 1, :], gel[:, 1, :], 0.044715, 1.0, ALU.mult, ALU.add, [gel], [gel])
                    S.tt(gel[:, 2, :], gel[:, 1, :], gel[:, 0, :], ALU.mult, [gel], [gel])
                    S.act(gel[:, 1, :], gel[:, 2, :], AF.Sigmoid, [gel], [gel], scale=GELU_C)
                    S.tt(gel[:, 2, :], gel[:, 0, :], gel[:, 1, :], ALU.mult, [gel], [gel])
                    gb = gab[u]
                    for hf in range(2):
                        S.tt(gb[:, hf * 4:(hf + 1) * 4, :], gel[:, 2, hf * 512:(hf + 1) * 512].rearrange("p (c t) -> p c t", t=128),
                             PB[hf][:].rearrange("p (c t) -> p c t", t=128), ALU.mult, [gel, PB[hf]], [gb])
                    for ii in range(8):
                        for hf in range(2):
                            S.mm(PB[4 + 2 * u + hf][:], gb[:, ii, :], vb[li][:, ii, hf * 512:(hf + 1) * 512],
                                 ec == 0 and ii == 0, ec == 15 and ii == 7, [gb, vb[li]], [PB[4 + 2 * u + hf]])
            for u in range(2):
                it = 2 * ip + u
                t0 = it * 128
                xb = xt[u]
                for hf in range(2):
                    S.tt(scr[:, hf * 512:(hf + 1) * 512], PB[4 + 2 * u + hf][:], GA2[:, hf * 512:(hf + 1) * 512], ALU.mult,
                         [PB[4 + 2 * u + hf], mod], [scr])
                xo_t = xb
                S.tt(xo_t[:], scr[:], xb[:], ALU.add, [scr, xb], [xo_t])
                if last:
                    fo = gel.view("fo", gel[:, 0, :])
                    fs = gel.view("fs", gel[:, 1, :])
                    rmsnorm_mod_tile(S, xo_t[:], xo_t, gfn, None, fo[:], fo, fs, small)
                    S.dma(xo.t[t0:t0 + 128, :], fo[:], reads=[fo], writes=[xo])
                else:
                    S.dma(xo.t[t0:t0 + 128, :], xo_t[:], reads=[xo_t], writes=[xo])
        S.emit_block(st)
        S.stack = S.outer
    return nc


def host_inputs_D2(x1_list, inp, l, S):
    wada = _f32(inp["w_ada"][l][:, 3072:6144])
    bada = _f32(np.broadcast_to(inp["b_ada"][l][None, 3072:6144], (128, 3072)))
    gff = _f32(np.broadcast_to(inp["g_ffn"][l][None, :], (128, D)))
    gfin = _f32(np.broadcast_to(inp["g_final"][None, :], (128, D)))
    kT = _f32(np.stack([inp["peer_k1"][l].T, inp["peer_k2"][l].T]))
    UT = _f32(inp["peer_u"][l].T)
    V = _f32(inp["peer_v"][l])
    wq = _f32(inp["peer_wq"][l])
    maps = []
    for cid in range(8):
        b = cid // 4
        maps.append({"xs": _f32(x1_list[cid]), "cT": _f32(inp["c"][b].reshape(8, 128).T), "wada": wada, "bada": bada,
                     "gffn": gff, "gfinal": gfin, "wq": wq, "kT": kT, "UT": UT, "V": V})
    return maps


def build_fused(SQ):
    nc = bass.Bass("TRN2", target_bir_lowering=False)
    NB = SQ // 256
    with ExitStack() as outer:
        S = Sched(nc, outer)
        x_in = S.din("x", [SQ, D], F32)
        cT = S.din("cT", [128, 8], F32)
        pos = S.din("pos", [16, SQ], F32)
        gfin = S.din("gfinal", [128, D], F32)
        out = S.dout("out", [SQ, D], F32)
        xcur = x_in
        for l in range(2):
            P = "L%d_" % l
            wada = S.din(P + "wada", [D, 6144], F32)
            bada = S.din(P + "bada", [128, 6144], F32)
            gat = S.din(P + "gattn", [128, D], F32)
            gff = S.din(P + "gffn", [128, D], F32)
            ext = {n: S.din(P + n, sh, F32) for n, sh in (
                ("wT", [D, NHT * 64]), ("wS", [D, 28 * 64]), ("wM", [D, NM]),
                ("peT", [2, 64, 32]), ("w1", [2, 2048, 128]), ("w2", [2, 128, 64]),
                ("wG", [D, 2048]), ("wupn", [512, D]), ("wupm", [512, D]), ("wout", [D, D]),
                ("wq", [D, 2048]), ("kT", [2, 128, 128]), ("UT", [D, 16384]), ("V", [16384, D]))}
            scr = {n: S.dscr(P + n, sh, F32) for n, sh in (
                ("oT", [NHT, 64, SQ // 2]), ("oQ", [8, 64, SQ // 2]), ("oM", [SQ, 384]), ("oG", [SQ, 24]),
                ("oK", [8, 64, NB]), ("oN", [SQ, 256]), ("oMo", [SQ, 256]), ("x1", [SQ, D]), ("x2", [SQ, D]))}
            ioA = dict(xs=xcur, cT=cT, wada=wada.view("wadaA", wada.t[:, 0:2048]), bada=bada.view("badaA", bada.t[:, 0:2048]),
                       gattn=gat, wT=ext["wT"], wS=ext["wS"], wM=ext["wM"], pos=pos,
                       oT=scr["oT"], oQ=scr["oQ"], oM=scr["oM"], oG=scr["oG"], oK=scr["oK"])
            build_stageA(SQ, S=S, io=ioA, pfx=P + "A_")
            for g in range(4):
                ioC = dict(oT=scr["oT"], oQ=scr["oQ"], oM=scr["oM"], oG=scr["oG"], oK=scr["oK"],
                           oN=scr["oN"], oMo=scr["oMo"], peT=ext["peT"], w1=ext["w1"], w2=ext["w2"])
                build_stageC(SQ, S=S, io=ioC, pfx=P + "C%d_" % g, g=g)
            ioD1 = dict(xs=xcur, oN=scr["oN"], oMo=scr["oMo"], cT=cT,
                        wada=wada.view("wadaD1", wada.t[:, 0:3072]), bada=bada.view("badaD1", bada.t[:, 0:3072]),
                        gattn=gat, wG=ext["wG"], wupn=ext["wupn"], wupm=ext["wupm"], wout=ext["wout"], xo=scr["x1"])
            build_stageD1(SQ, S=S, io=ioD1, pfx=P + "D1_")
            last = (l == 1)
            ioD2 = dict(xs=scr["x1"], cT=cT, wada=wada.view("wadaD2", wada.t[:, 3072:6144]),
                        bada=bada.view("badaD2", bada.t[:, 3072:6144]), gffn=gff, gfinal=gfin,
                        wq=ext["wq"], kT=ext["kT"], UT=ext["UT"], V=ext["V"], xo=(out if last else scr["x2"]))
            build_stageD2(SQ, last, S=S, io=ioD2, pfx=P + "D2_")
            xcur = scr["x2"]
    return nc


def host_inputs_fused(inp, S):
    maps = []
    per_layer = []
    for l in range(2):
        w_in = inp["w_in"][l]
        wT = np.concatenate([w_in[:, c:c + 64] for c in HT_COLS], axis=1)
        sw = []
        for i in ROPE_IDX:
            c = HT_COLS[i]
            sw += [w_in[:, c + 8:c + 16], w_in[:, c:c + 8], np.zeros((D, 48), np.float32)]
        P = "L%d_" % l
        per_layer.append({
            P + "wada": _f32(inp["w_ada"][l]), P + "bada": _f32(np.broadcast_to(inp["b_ada"][l][None, :], (128, 6144))),
            P + "gattn": _f32(np.broadcast_to(inp["g_attn"][l][None, :], (128, D))),
            P + "gffn": _f32(np.broadcast_to(inp["g_ffn"][l][None, :], (128, D))),
            P + "wT": _f32(wT), P + "wS": _f32(np.concatenate(sw, axis=1)),
            P + "wM": _f32(np.concatenate([w_in[:, C_VS:C_VS + 128], w_in[:, C_VW:C_VW + 128], w_in[:, C_MV:C_MV + 512],
                                           w_in[:, C_NG:C_NG + 24]], axis=1)),
            P + "peT": _f32(np.transpose(inp["cmp_pe"][l], (0, 2, 1))), P + "w1": _f32(inp["cmp_w1"][l]),
            P + "w2": _f32(inp["cmp_w2"][l]), P + "wG": _f32(w_in[:, C_GN:C_GN + 2048]),
            P + "wupn": _f32(inp["w_up_nsa"][l]), P + "wupm": _f32(inp["w_up_moba"][l]), P + "wout": _f32(inp["w_out"][l]),
            P + "wq": _f32(inp["peer_wq"][l]), P + "kT": _f32(np.stack([inp["peer_k1"][l].T, inp["peer_k2"][l].T])),
            P + "UT": _f32(inp["peer_u"][l].T), P + "V": _f32(inp["peer_v"][l]),
        })
    pos = _f32(np.broadcast_to(np.arange(S, dtype=np.float32)[None, :], (16, S)))
    gfin = _f32(np.broadcast_to(inp["g_final"][None, :], (128, D)))
    for cid in range(8):
        b = cid // 4
        m = {"x": _f32(inp["x"][b]), "cT": _f32(inp["c"][b].reshape(8, 128).T), "pos": pos, "gfinal": gfin}
        m.update(per_layer[0]); m.update(per_layer[1])
        maps.append(m)
    return maps


_PROGS = {}


def _prog(key, builder):
    if key not in _PROGS:
        _PROGS[key] = builder()
    return _PROGS[key]


def _run(nc, maps):
    res = run_bass_kernel_spmd(nc, maps, core_ids=list(range(8)))
    return [{k: np.asarray(v) for k, v in r.items()} for r in res.results]


FUSED = False


def kernel(**inputs):
    inp = {k: np.asarray(v) for k, v in inputs.items()}
    B, S, _ = inp["x"].shape
    assert B == 2
    if FUSED:
        res = _run(_prog(("F", S), lambda: build_fused(S)), host_inputs_fused(inp, S))
        return np.ascontiguousarray(np.stack([res[0]["out"], res[4]["out"]]), dtype=np.float32)
    TL = S // 4
    x = np.asarray(inp["x"], dtype=np.float32)
    for l in range(2):
        resA = _run(_prog(("A", TL), lambda: build_stageA(TL)), host_inputs_A(inp, l, x, S))
        resC = _run(_prog(("C", S), lambda: build_stageC(S)), host_inputs_C(resA, inp, l, S))
        del resA
        resD1 = _run(_prog(("D1", TL), lambda: build_stageD1(TL)), host_inputs_D1(resC, inp, l, x, S))
        del resC
        x1 = [r["xo"] for r in resD1]
        last = (l == 1)
        resD2 = _run(_prog(("D2", TL, last), lambda: build_stageD2(TL, last)), host_inputs_D2(x1, inp, l, S))
        x = np.stack([np.concatenate([resD2[4 * b + g]["xo"] for g in range(4)], axis=0) for b in range(2)])
    return np.ascontiguousarray(x, dtype=np.float32)
```

```python
import math
import numpy as np
import ml_dtypes
from contextlib import ExitStack
import concourse.bass as bass
import concourse.mybir as mybir
from concourse.bass_utils import run_bass_kernel_spmd

F32 = mybir.dt.float32
BF16 = mybir.dt.bfloat16
I32 = mybir.dt.int32
U32 = mybir.dt.uint32
AF = mybir.ActivationFunctionType
ALU = mybir.AluOpType
AX = mybir.AxisListType

NDMASEM = 24
SEMLIM = 1 << 28
COMPUTE = ("act", "dve", "pool", "pe")

D = 1024
NEG = -1.0e30
EPS = 1e-6
ROPE_THETA = 500000.0


class Buf:
    __slots__ = ("name", "t", "last_w", "readers", "excl", "parent")

    def __init__(self, name, t=None, excl=False, parent=None):
        self.name = name
        self.t = t
        self.last_w = None
        self.readers = []
        self.excl = excl
        self.parent = parent

    def view(self, name, ap):
        return Buf(name, ap, excl=self.excl, parent=self if self.parent is None else self.parent)

    def __getitem__(self, k):
        return self.t[k]


class Sched:
    def __init__(self, nc, stack):
        self.nc = nc
        self.stack = stack
        self.outer = stack
        self.ops = []
        self.pfx = ""
        self.emitted = 0
        self.cnt = {e: 0 for e in COMPUTE}
        self.ndma = 0
        self.sems = {}
        self.last_eng = {}
        self.recent_dma = []

    def sbuf(self, name, shape, dtype):
        t = self.stack.enter_context(self.nc.sbuf_tensor("sb_" + self.pfx + name, list(shape), dtype))
        return Buf(name, t)

    def psum(self, name, shape, dtype):
        t = self.stack.enter_context(self.nc.psum_tensor("ps_" + self.pfx + name, list(shape), dtype))
        return Buf(name, t, excl=True)

    def din(self, name, shape, dtype):
        return Buf(name, self.nc.dram_tensor(name, list(shape), dtype, kind="ExternalInput"))

    def dout(self, name, shape, dtype):
        return Buf(name, self.nc.dram_tensor(name, list(shape), dtype, kind="ExternalOutput"))

    def dscr(self, name, shape, dtype):
        return Buf(name, self.nc.dram_tensor(name, list(shape), dtype, kind="Internal"))

    def op(self, eng, fn, reads=(), writes=(), dma=False):
        i = len(self.ops)
        deps = set()
        reads = [b if b.parent is None else b.parent for b in reads]
        writes = [b if b.parent is None else b.parent for b in writes]
        ex = [b for b in reads if b.excl and b not in writes]
        if ex:
            writes = list(writes) + ex
        for b in reads:
            if b.last_w is not None:
                deps.add(b.last_w)
        for b in writes:
            if b.last_w is not None:
                deps.add(b.last_w)
            for r in b.readers:
                deps.add(r)
        deps.discard(i)
        for b in reads:
            b.readers.append(i)
        for b in writes:
            b.last_w = i
            b.readers = []
        self.ops.append(dict(eng=eng, fn=fn, deps=deps, dma=dma, signal=False))
        if dma:
            self.recent_dma = (self.recent_dma + [i])[-NDMASEM:]
        else:
            self.last_eng[eng] = i
        return i

    def barrier(self):
        deps = set(self.last_eng.values()) | set(self.recent_dma)
        for e in ("sp", "act", "dve", "pool", "pe"):
            i = self.op(e, lambda eng: eng.nop(), (), ())
            self.ops[i]["deps"] |= deps

    def dma(self, out, in_, reads=(), writes=(), eng="sp", **kw):
        return self.op(eng, lambda e: e.dma_start(out=out, in_=in_, **kw), reads, writes, dma=True)

    def mm(self, out, lhsT, rhs, start, stop, reads, writes):
        return self.op("pe", lambda e: e.matmul(out, lhsT=lhsT, rhs=rhs, start=start, stop=stop,
                                                skip_group_check=True), reads, writes)

    def tr(self, out, in_, ident, reads, writes):
        return self.op("pe", lambda e: e.transpose(out, in_, ident), reads, writes)

    def act(self, out, in_, func, reads, writes, **kw):
        return self.op("act", lambda e: e.activation(out=out, in_=in_, func=func, **kw), reads, writes)

    def tt(self, out, in0, in1, op, reads, writes, eng="dve"):
        return self.op(eng, lambda e: e.tensor_tensor(out=out, in0=in0, in1=in1, op=op), reads, writes)

    def ts(self, out, in0, s1, s2, op0, op1, reads, writes, eng="dve", **kw):
        if op1 is None:
            return self.op(eng, lambda e: e.tensor_scalar(out=out, in0=in0, scalar1=s1, scalar2=None,
                                                          op0=op0, **kw), reads, writes)
        return self.op(eng, lambda e: e.tensor_scalar(out=out, in0=in0, scalar1=s1, scalar2=s2,
                                                      op0=op0, op1=op1, **kw), reads, writes)

    def stt(self, out, in0, scalar, in1, op0, op1, reads, writes, **kw):
        return self.op("dve", lambda e: e.scalar_tensor_tensor(out=out, in0=in0, scalar=scalar, in1=in1,
                                                               op0=op0, op1=op1, **kw), reads, writes)

    def copy(self, out, in_, reads, writes, eng="dve"):
        if eng == "act":
            return self.op("act", lambda e: e.copy(out=out, in_=in_), reads, writes)
        return self.op(eng, lambda e: e.tensor_copy(out=out, in_=in_), reads, writes)

    def memset(self, ap, val, writes, eng="pool"):
        return self.op(eng, lambda e: e.memset(ap, val), (), writes)

    def emit_block(self, stack):
        self.barrier()
        nc = self.nc
        ops = self.ops
        lo, hi = self.emitted, len(ops)
        for i in range(lo, hi):
            ops[i]["deps"] = {d for d in ops[i]["deps"] if d >= lo}
        for i in range(lo, hi):
            o = ops[i]
            for d in o["deps"]:
                po = ops[d]
                if po["eng"] == "pe" and o["eng"] == "pe" and not po["dma"] and not o["dma"]:
                    continue
                po["signal"] = True
        per_gen = NDMASEM * (SEMLIM // 16)
        for i in range(lo, hi):
            o = ops[i]
            if o["dma"]:
                g = self.ndma // per_gen
                r = self.ndma % per_gen
                o["sem"] = ("dma", r % NDMASEM, g)
                o["tgt"] = 16 * (r // NDMASEM + 1)
                self.ndma += 1
            elif o["signal"]:
                g = self.cnt[o["eng"]] // SEMLIM
                o["sem"] = ("c", o["eng"], g)
                o["tgt"] = self.cnt[o["eng"]] % SEMLIM + 1
                self.cnt[o["eng"]] += 1
            else:
                continue
            if o["sem"] not in self.sems:
                k = o["sem"]
                self.sems[k] = self.outer.enter_context(nc.semaphore("s_" + "_".join(str(x) for x in k)))
        sems = self.sems
        block = stack.enter_context(nc.Block())
        engs = ["sp", "act", "dve", "pool", "pe"]
        per = {e: [] for e in engs}
        for i in range(lo, hi):
            per[ops[i]["eng"]].append(i)

        def run(ename, eobj):
            known = {}
            for i in per[ename]:
                o = ops[i]
                waits = {}
                for d in o["deps"]:
                    po = ops[d]
                    if (not po["dma"]) and (not o["dma"]) and po["eng"] == "pe" and ename == "pe":
                        continue
                    if not (po["dma"] or po["signal"]):
                        continue
                    k = po["sem"]
                    waits[k] = max(waits.get(k, 0), po["tgt"])
                if o["dma"]:
                    k = o["sem"]
                    prev = o["tgt"] - 16
                    if prev > 0:
                        waits[k] = max(waits.get(k, 0), prev)
                for k, v in waits.items():
                    if known.get(k, 0) >= v:
                        continue
                    eobj.wait_ge(sems[k], v)
                    known[k] = v
                ins = o["fn"](eobj)
                if o["dma"]:
                    ins.then_inc(sems[o["sem"]], 16)
                elif o["signal"]:
                    ins.then_inc(sems[o["sem"]], 1)
                o["fn"] = None

        @block.sync
        def _(e):
            run("sp", e)

        @block.scalar
        def _(e):
            run("act", e)

        @block.vector
        def _(e):
            run("dve", e)

        @block.gpsimd
        def _(e):
            run("pool", e)

        @block.tensor
        def _(e):
            run("pe", e)

        self.emitted = hi


def make_ident(S, dtype, name):
    ones = S.sbuf(name + "_ones", [128, 128], F32)
    idt = S.sbuf(name, [128, 128], dtype)
    S.memset(ones[:], 1.0, [ones])
    S.op("pool", lambda e: e.affine_select(out=idt[:], in_=ones[:], pattern=[[1, 128]],
                                           compare_op=ALU.is_equal, fill=0.0, base=0,
                                           channel_multiplier=-1), [ones], [idt])
    return idt


def load_cast_weight(S, dst, dst_col0, src, ncols, stage, K=8):
    c0 = 0
    while c0 < ncols:
        n = min(512, ncols - c0)
        S.dma(stage[:, :, 0:n], src.t[:, c0:c0 + n].rearrange("(k p) n -> p k n", p=128), writes=[stage])
        S.copy(dst[:, :, dst_col0 + c0:dst_col0 + c0 + n], stage[:, :, 0:n], [stage], [dst], eng="pool")
        c0 += n


def compute_mod(S, cT_d, wada_d, bada_d, ncols, name, wst, ps=None):
    cT = S.sbuf(name + "_cT", [128, 8], F32)
    S.dma(cT[:], cT_d.t[:, :], writes=[cT])
    sT = S.sbuf(name + "_sT", [128, 8], F32)
    S.act(sT[:], cT[:], AF.Silu, [cT], [sT])
    lhs = S.sbuf(name + "_lhs", [128, 8, 128], F32)
    for k in range(8):
        S.copy(lhs[:, k, :], sT[:, k:k + 1].to_broadcast([128, 128]), [sT], [lhs])
    mod = S.sbuf(name, [128, ncols], F32)
    bst = S.sbuf(name + "_bst", [128, 512], F32)
    if ps is None:
        ps = S.psum(name + "_ps", [128, 512], F32)
    for c0 in range(0, ncols, 512):
        S.dma(wst[:], wada_d.t[:, c0:c0 + 512].rearrange("(k p) n -> p k n", p=128), writes=[wst])
        S.dma(bst[:], bada_d.t[:, c0:c0 + 512], writes=[bst])
        for k in range(8):
            S.mm(ps[:], lhs[:, k, :], wst[:, k, :], k == 0, k == 7, [lhs, wst], [ps])
        S.tt(mod[:, c0:c0 + 512], ps[:], bst[:], ALU.add, [ps, bst], [mod])
    return mod


def rmsnorm_mod_tile(S, x_ap, xbuf, A, Bm, out_ap, outbuf, scr, small, Bbuf=None):
    S.act(scr[:], x_ap, AF.Square, [xbuf], [scr, small], accum_out=small[:, 0:1])
    S.ts(small[:, 1:2], small[:, 0:1], 1.0 / D, EPS, ALU.mult, ALU.add, [small], [small])
    S.act(small[:, 2:3], small[:, 1:2], AF.Sqrt, [small], [small])
    S.op("dve", lambda e: e.reciprocal(out=small[:, 3:4], in_=small[:, 2:3]), [small], [small])
    if Bm is None:
        S.stt(out_ap, x_ap, small[:, 3:4], A[:], ALU.mult, ALU.mult, [xbuf, small, A], [outbuf])
    else:
        S.stt(scr[:], x_ap, small[:, 3:4], A[:], ALU.mult, ALU.mult, [xbuf, small, A], [scr])
        S.tt(out_ap, scr[:], Bm, ALU.add, [scr, Bbuf], [outbuf])


ROTENG = "dve"
NHT = 32
ROPE_IDX = list(range(0, 8)) + [12, 13, 14, 15] + list(range(16, 32))
NM = 792


def build_stageA(TL, level=99, S=None, io=None, pfx=""):
    alone = S is None
    nc = bass.Bass("TRN2", target_bir_lowering=False) if alone else S.nc
    NCH = TL // 512
    with ExitStack() as st:
        if alone:
            S = Sched(nc, st)
        S.stack = st
        S.pfx = pfx
        _in = (lambda n, sh, dt: S.din(n, sh, dt)) if alone else (lambda n, sh, dt: io[n])
        _out = (lambda n, sh, dt: S.dout(n, sh, dt)) if alone else (lambda n, sh, dt: io[n])
        xs = _in("xs", [TL, D], F32)
        cT_d = _in("cT", [128, 8], F32)
        wada = _in("wada", [D, 2048], F32)
        bada = _in("bada", [128, 2048], F32)
        gat = _in("gattn", [128, D], F32)
        wT_d = _in("wT", [D, NHT * 64], F32)
        wS_d = _in("wS", [D, 28 * 64], F32)
        wM_d = _in("wM", [D, NM], F32)
        pos_d = _in("pos", [16, TL], F32)
        oT = _out("oT", [NHT, 64, TL // 2], F32)
        oQ = _out("oQ", [8, 64, TL // 2], F32)
        oM = _out("oM", [TL, 384], F32)
        oG = _out("oG", [TL, 24], F32)
        oK = _out("oK", [8, 64, TL // 256], F32)

        identb = make_ident(S, BF16, "identb")
        wT = S.sbuf("wTb", [128, 8, NHT * 64], BF16)
        wS = S.sbuf("wSb", [128, 8, 28 * 64], BF16)
        wM = S.sbuf("wMb", [128, 8, NM], BF16)
        stage = S.sbuf("wstage", [128, 8, 512], F32)
        load_cast_weight(S, wT, 0, wT_d, NHT * 64, stage)
        load_cast_weight(S, wS, 0, wS_d, 28 * 64, stage)
        load_cast_weight(S, wM, 0, wM_d, NM, stage)
        if level <= 1:
            S.emit_block(st); return nc
        mod = compute_mod(S, cT_d, wada, bada, 2048, "modA", stage)
        gt = S.sbuf("gat", [128, D], F32)
        S.dma(gt[:], gat.t[:, :], writes=[gt])
        A1 = S.sbuf("A1", [128, D], F32)
        S.stt(A1[:], mod[:, 1024:2048], 1.0, gt[:], ALU.add, ALU.mult, [mod, gt], [A1])
        B1 = mod[:, 0:1024]
        if level <= 2:
            S.emit_block(st); return nc
        pidx_i = S.sbuf("pidx_i", [16, 1], I32)
        S.op("pool", lambda e: e.iota(pidx_i[:], pattern=[[0, 1]], base=0, channel_multiplier=1), [], [pidx_i])
        pf = S.sbuf("pf", [16, 4], F32)
        S.copy(pf[:, 0:1], pidx_i[:], [pidx_i], [pf])
        S.ts(pf[:, 3:4], pf[:, 0:1], 8.0, 2.0, ALU.is_ge, ALU.mult, [pf], [pf])
        S.stt(pf[:, 1:2], pf[:, 3:4], -4.0, pf[:, 0:1], ALU.mult, ALU.add, [pf], [pf])
        S.ts(pf[:, 3:4], pf[:, 3:4], -1.0, None, ALU.add, None, [pf], [pf])
        S.act(pf[:, 2:3], pf[:, 1:2], AF.Exp, [pf], [pf], scale=-math.log(ROPE_THETA) / 8.0)
        S.ts(pf[:, 2:3], pf[:, 2:3], 1.0 / (2 * math.pi), None, ALU.mult, None, [pf], [pf])

        if level <= 3:
            S.emit_block(st); return nc
        xt = [S.sbuf("xt%d" % i, [128, D], F32) for i in range(2)]
        scr = S.sbuf("scr", [128, D], F32)
        small = S.sbuf("small", [128, 4], F32)
        hb = S.sbuf("hb", [128, D], BF16)
        hT = S.sbuf("hT", [128, 8, 512], BF16)
        pstr = S.psum("pstr", [128, 8, 128], BF16)
        psh = [S.psum("psh%d" % i, [64, 512], F32) for i in range(2)]
        pss = [S.psum("pss%d" % i, [64, 512], F32) for i in range(2)]
        psm = [S.psum("psm%d" % i, [128, 512], F32) for i in range(2)]
        outT = S.sbuf("outT", [64, NHT, 512], BF16)
        outQ = S.sbuf("outQ", [64, 8, 512], BF16)
        outM = S.sbuf("outM", [128, 768], BF16)
        outG = S.sbuf("outG", [128, 24], F32)
        km = S.sbuf("km", [64, 8, 2], F32)
        posr = S.sbuf("posr", [16, 512], F32)
        ry = S.sbuf("ry", [16, 512], F32)
        ryi = S.sbuf("ryi", [16, 512], I32)
        ryf = S.sbuf("ryf", [16, 512], F32)
        C16 = S.sbuf("C16", [16, 512], F32)
        S16 = S.sbuf("S16", [16, 512], F32)
        t1 = S.sbuf("t1", [16, 512], F32)
        t2 = S.sbuf("t2", [16, 512], F32)
        rot = S.sbuf("rot", [16, 512], F32)

        def frac_sin(dst, add):
            S.ts(ryf[:], ry[:], add, None, ALU.add, None, [ry], [ryf])
            S.copy(ryi[:], ryf[:], [ryf], [ryi])
            S.copy(t1[:], ryi[:], [ryi], [t1])
            S.tt(t2[:], ryf[:], t1[:], ALU.subtract, [ryf, t1], [t2])
            S.stt(t1[:], t2[:], 0.5, t2[:], ALU.is_gt, ALU.subtract, [t2], [t1])
            S.stt(t2[:], t1[:], 0.5, t1[:], ALU.is_gt, ALU.subtract, [t1], [t2])
            S.act(dst[:], t2[:], AF.Sin, [t2], [dst], scale=2 * math.pi)

        for ch in range(NCH):
            t0 = ch * 512
            S.dma(posr[:], pos_d.t[:, t0:t0 + 512], writes=[posr])
            S.ts(ry[:], posr[:], pf[:, 2:3], None, ALU.mult, None, [posr, pf], [ry])
            frac_sin(S16, 0.0)
            S.ts(S16[:], S16[:], pf[:, 3:4], None, ALU.mult, None, [S16, pf], [S16])
            frac_sin(C16, 0.25)
            if level <= 4:
                continue
            for tt_ in range(4):
                xb = xt[tt_ % 2]
                S.dma(xb[:], xs.t[t0 + tt_ * 128:t0 + (tt_ + 1) * 128, :], reads=[xs], writes=[xb])
                rmsnorm_mod_tile(S, xb[:], xb, A1, B1, hb[:], hb, scr, small, Bbuf=mod)
                for k in range(8):
                    S.tr(pstr[:, k, :], hb[:, k * 128:(k + 1) * 128], identb[:], [hb, identb], [pstr])
                S.copy(hT[:, :, tt_ * 128:(tt_ + 1) * 128], pstr[:], [pstr], [hT], eng="act")
                for half, (c0, c1) in enumerate(((0, 512), (512, NM))):
                    pm = psm[half]
                    for k in range(8):
                        S.mm(pm[:, 0:c1 - c0], hT[:, k, tt_ * 128:(tt_ + 1) * 128], wM[:, k, c0:c1],
                             k == 0, k == 7, [hT, wM], [pm])
                S.copy(outM[:, 0:512], psm[0][:, 0:512], [psm[0]], [outM], eng="act")
                S.copy(outM[:, 512:768], psm[1][:, 0:256], [psm[1]], [outM], eng="act")
                S.act(outG[:], psm[1][:, 256:280], AF.Sigmoid, [psm[1]], [outG])
                S.dma(oM.t[t0 + tt_ * 128:t0 + (tt_ + 1) * 128, :], outM[:].bitcast(F32), reads=[outM], writes=[oM])
                S.dma(oG.t[t0 + tt_ * 128:t0 + (tt_ + 1) * 128, :], outG[:], reads=[outG], writes=[oG])
            for i in range(NHT if level > 5 else 0):
                ph = psh[i % 2]
                for k in range(8):
                    S.mm(ph[:], wT[:, k, i * 64:(i + 1) * 64], hT[:, k, :], k == 0, k == 7, [wT, hT], [ph])
                S.copy(outT[:, i, :], ph[:], [ph], [outT], eng="act")
                if i < 8:
                    S.copy(outQ[:, i, :], ph[:], [ph], [outQ], eng="act")
                if i >= 24 and level > 7:
                    S.op("dve", lambda e, ph=ph, i=i: e.tensor_reduce(
                        out=km[:, i - 24, :], in_=ph[:].rearrange("p (b t) -> p b t", t=256),
                        axis=AX.X, op=ALU.add), [ph], [km])
                if i in ROPE_IDX and level > 6.05:
                    r = ROPE_IDX.index(i)
                    p2 = pss[r % 2]
                    for k in range(8):
                        S.mm(p2[:], wS[:, k, r * 64:(r + 1) * 64], hT[:, k, :], k == 0, k == 7, [wS, hT], [p2])
                    if level > 6.15:
                        S.tt(t1[:], ph[0:16, :], C16[:], ALU.mult, [ph, C16], [t1])
                    if level > 6.25:
                        S.tt(t2[:], p2[0:16, :], S16[:], ALU.mult, [p2, S16], [t2])
                    if level > 6.35:
                        S.tt(rot[:], t1[:], t2[:], ALU.add, [t1, t2], [rot])
                    if level > 6.45:
                        S.copy(outT[0:16, i, :], rot[:], [rot], [outT], eng=ROTENG)
                    if i >= 24 and level > 7:
                        S.op("dve", lambda e, i=i: e.tensor_reduce(
                            out=km[0:16, i - 24, :], in_=rot[:].rearrange("p (b t) -> p b t", t=256),
                            axis=AX.X, op=ALU.add), [rot], [km])
            if level > 7:
                S.ts(km[:], km[:], 1.0 / 256.0, None, ALU.mult, None, [km], [km])
            S.dma(oT.t[:, :, t0 // 2:t0 // 2 + 256].rearrange("i d t -> d i t"), outT[:].bitcast(F32), reads=[outT], writes=[oT])
            S.dma(oQ.t[:, :, t0 // 2:t0 // 2 + 256].rearrange("i d t -> d i t"), outQ[:].bitcast(F32), reads=[outQ], writes=[oQ])
            if level > 7:
                S.dma(oK.t[:, :, ch * 2:ch * 2 + 2].rearrange("h d b -> d h b"), km[:], reads=[km], writes=[oK])
        S.emit_block(st)
        S.stack = S.outer
    return nc


C_NQ, C_KC, C_VC, C_KS, C_VS, C_KW, C_VW, C_NG, C_MQ, C_MK, C_MV, C_GN, C_GM = (
    0, 512, 640, 768, 896, 1024, 1152, 1280, 1304, 1816, 2328, 2840, 3864)
HT_COLS = ([C_NQ + 64 * h for h in range(8)] + [C_KC, C_KC + 64] + [C_VC, C_VC + 64] + [C_KS, C_KS + 64]
           + [C_KW, C_KW + 64] + [C_MQ + 64 * h for h in range(8)] + [C_MK + 64 * h for h in range(8)])


def _f32(a):
    return np.ascontiguousarray(a, dtype=np.float32)


def host_inputs_A(inp, l, x_cur, S):
    TL = S // 4
    w_in = inp["w_in"][l]
    wT = np.concatenate([w_in[:, c:c + 64] for c in HT_COLS], axis=1)
    sw = []
    for i in ROPE_IDX:
        c = HT_COLS[i]
        sw.append(w_in[:, c + 8:c + 16])
        sw.append(w_in[:, c:c + 8])
        sw.append(np.zeros((D, 48), np.float32))
    wS = np.concatenate(sw, axis=1)
    wM = np.concatenate([w_in[:, C_VS:C_VS + 128], w_in[:, C_VW:C_VW + 128], w_in[:, C_MV:C_MV + 512],
                         w_in[:, C_NG:C_NG + 24]], axis=1)
    wada = inp["w_ada"][l][:, 0:2048]
    bada = np.broadcast_to(inp["b_ada"][l][None, 0:2048], (128, 2048))
    gat = np.broadcast_to(inp["g_attn"][l][None, :], (128, D))
    maps = []
    for cid in range(8):
        b, g = cid // 4, cid % 4
        pos = np.broadcast_to(np.arange(g * TL, (g + 1) * TL, dtype=np.float32)[None, :], (16, TL))
        maps.append({
            "xs": _f32(x_cur[b, g * TL:(g + 1) * TL]),
            "cT": _f32(inp["c"][b].reshape(8, 128).T),
            "wada": _f32(wada), "bada": _f32(bada), "gattn": _f32(gat),
            "wT": _f32(wT), "wS": _f32(wS), "wM": _f32(wM), "pos": _f32(pos),
        })
    return maps


SCALE = 0.125
MASKBIG = 240000.0
GELU_C = 1.5957691216057308


def gelu_tanh(S, out_ap, outbuf, x, sq, tmp, n):
    S.act(sq[:, 0:n], x[:, 0:n], AF.Square, [x], [sq])
    S.ts(sq[:, 0:n], sq[:, 0:n], 0.044715, 1.0, ALU.mult, ALU.add, [sq], [sq])
    S.tt(tmp[:, 0:n], sq[:, 0:n], x[:, 0:n], ALU.mult, [sq, x], [tmp])
    S.act(sq[:, 0:n], tmp[:, 0:n], AF.Sigmoid, [tmp], [sq], scale=GELU_C)
    S.tt(out_ap, x[:, 0:n], sq[:, 0:n], ALU.mult, [x, sq], [outbuf])


def build_stageC(SQ, S=None, io=None, pfx="", g=None):
    alone = S is None
    nc = bass.Bass("TRN2", target_bir_lowering=False) if alone else S.nc
    NQC = SQ // 512
    NKT = SQ // 128
    NB = SQ // 256
    NCMP = SQ // 16 - 1
    NCT = (NCMP + 127) // 128
    with ExitStack() as st:
        if alone:
            S = Sched(nc, st)
        S.stack = st
        S.pfx = pfx
        _in = (lambda n, sh, dt: S.din(n, sh, dt)) if alone else (lambda n, sh, dt: io[n])
        _out = (lambda n, sh, dt: S.dout(n, sh, dt)) if alone else (lambda n, sh, dt: io[n])
        if alone:
            qraw_d = _in("qraw", [4, 64, SQ // 2], F32)
            qrot_d = _in("qrot", [2, 64, SQ // 2], F32)
            kcp_d = _in("kcp", [2, 64, SQ // 2], F32)
            ks_d = _in("ksT", [64, SQ // 2], F32)
            kw_d = _in("kwT", [64, SQ // 2], F32)
            vsw_d = _in("vsw", [SQ, 64], F32)
            gates_d = _in("gates", [SQ, 6], F32)
            mq_d = _in("mq", [2, 64, SQ // 2], F32)
            mk_d = _in("mk", [2, 64, SQ // 2], F32)
            mv_d = _in("mv", [SQ, 64], F32)
            km_d = _in("kmean", [2, 64, NB], F32)
            onsa_d = _out("onsa", [SQ, 64], F32)
            omoba_d = _out("omoba", [SQ, 64], F32)
            qrawA = qraw_d.view("qrawA", qraw_d.t[0:2])
            qrawB = qraw_d.view("qrawB", qraw_d.t[2:4])
            vs_d = vsw_d.view("vs_d", vsw_d.t[:, 0:32])
            vw_d = vsw_d.view("vw_d", vsw_d.t[:, 32:64])
        else:
            kv = g // 2
            oth0 = 4 * kv + (2 if g % 2 == 0 else 0)
            oT_, oQ_, oM_, oG_, oK_ = io["oT"], io["oQ"], io["oM"], io["oG"], io["oK"]
            qrawA = oQ_.view("qrawA", oQ_.t[2 * g:2 * g + 2])
            qrawB = oQ_.view("qrawB", oQ_.t[oth0:oth0 + 2])
            qrot_d = oT_.view("qrot", oT_.t[2 * g:2 * g + 2])
            kcp_d = oT_.view("kcp", oT_.t[8 + kv:12:2])
            ks_d = oT_.view("ksT", oT_.t[12 + kv])
            kw_d = oT_.view("kwT", oT_.t[14 + kv])
            vs_d = oM_.view("vs_d", oM_.t[:, 32 * kv:32 * kv + 32])
            vw_d = oM_.view("vw_d", oM_.t[:, 64 + 32 * kv:64 + 32 * kv + 32])
            gates_d = oG_.view("gates", oG_.t[:, 6 * g:6 * g + 6])
            mq_d = oT_.view("mq", oT_.t[16 + 2 * g:18 + 2 * g])
            mk_d = oT_.view("mk", oT_.t[24 + 2 * g:26 + 2 * g])
            mv_d = oM_.view("mv", oM_.t[:, 128 + 64 * g:128 + 64 * g + 64])
            km_d = oK_.view("kmean", oK_.t[2 * g:2 * g + 2])
            onsa_d = io["oN"].view("onsa", io["oN"].t[:, 64 * g:64 * g + 64])
            omoba_d = io["oMo"].view("omoba", io["oMo"].t[:, 64 * g:64 * g + 64])
        peT_d = _in("peT", [2, 64, 32], F32)
        w1_d = _in("w1", [2, 2048, 128], F32)
        w2_d = _in("w2", [2, 128, 64], F32)

        identf = make_ident(S, F32, "identf")
        identb = make_ident(S, BF16, "identb")
        PB = [S.psum("PB%d" % i, [128, 512], F32) for i in range(8)]

        onesb = S.sbuf("onesb", [128, 1024], BF16)
        S.memset(onesb[:], 1.0, [onesb])
        Ex = S.sbuf("Ex", [128, 64, 128], BF16)
        for e0 in range(0, 64, 8):
            S.op("pool", lambda e, e0=e0: e.affine_select(
                out=Ex[:, e0:e0 + 8, :], in_=onesb[:].rearrange("p (a k) -> p a k", k=128),
                pattern=[[-2, 8], [-1, 2], [0, 64]], compare_op=ALU.is_equal, fill=0.0,
                base=-2 * e0, channel_multiplier=1), [onesb], [Ex])
        Rm = S.sbuf("Rm", [64, 64, 128], BF16)
        for e0 in range(0, 64, 8):
            S.op("pool", lambda e, e0=e0: e.affine_select(
                out=Rm[:, e0:e0 + 8, :], in_=onesb[0:64, :].rearrange("p (a k) -> p a k", k=128),
                pattern=[[-1, 8], [0, 128]], compare_op=ALU.is_equal, fill=0.0,
                base=-e0, channel_multiplier=1), [onesb], [Rm])
        vcx = S.sbuf("vcx", [128, NCT, 321], BF16)
        S.memset(vcx[:], 0.0, [vcx])
        S.memset(vcx[:, :, 320:321], 1.0, [vcx])
        ia = S.sbuf("ia", [128, 256], I32)
        ib = S.sbuf("ib", [128, 256], I32)
        fa = S.sbuf("fa", [128, 256], F32)
        fb = S.sbuf("fb", [128, 256], F32)
        fc = S.sbuf("fc", [128, 256], F32)
        fd = S.sbuf("fd", [128, 256], F32)
        S.op("pool", lambda e: e.iota(ib[:], pattern=[[64, 256]], base=0, channel_multiplier=0), [], [ib])
        S.copy(fb[:], ib[:], [ib], [fb])
        for ct in range(NCT):
            S.op("pool", lambda e, ct=ct: e.iota(ia[:], pattern=[[0, 256]], base=2048 * ct, channel_multiplier=16), [], [ia])
            S.copy(fa[:], ia[:], [ia], [fa])
            S.ts(fc[:], fa[:], 32.0, None, ALU.add, None, [fa], [fc])
            S.stt(fc[:], fb[:], 64.0, fc[:], ALU.add, ALU.min, [fb, fc], [fc])
            S.tt(fd[:], fa[:], fb[:], ALU.max, [fa, fb], [fd])
            S.tt(fc[:], fc[:], fd[:], ALU.subtract, [fc, fd], [fc])
            S.ts(vcx[:, ct, 0:256], fc[:], 0.0, 1.0 / 32.0, ALU.max, ALU.mult, [fc], [vcx])

        kcT = S.sbuf("kcT", [64, NCT * 128], BF16)
        S.memset(kcT[:], 0.0, [kcT])
        w1b = S.sbuf("w1b", [64, 2, 32, 128], BF16)
        w1st = S.sbuf("w1st", [64, 32, 128], F32)
        w2b = S.sbuf("w2b", [128, 2, 64], BF16)
        w2st = S.sbuf("w2st", [128, 2, 64], F32)
        peTb = S.sbuf("peTb", [64, 2, 32], BF16)
        peTst = S.sbuf("peTst", [64, 2, 32], F32)
        for j in range(2):
            S.dma(w1st[:], w1_d.t[j].rearrange("(l d) h -> d l h", d=64), writes=[w1st])
            S.copy(w1b[:, j, :, :], w1st[:], [w1st], [w1b], eng="pool")
        S.dma(w2st[:], w2_d.t[:, :, :].rearrange("j h d -> h j d"), writes=[w2st])
        S.copy(w2b[:], w2st[:], [w2st], [w2b])
        S.dma(peTst[:], peT_d.t[:, :, :].rearrange("j d l -> d j l"), writes=[peTst])
        S.copy(peTb[:], peTst[:], [peTst], [peTb])
        CCH = min(512, NCT * 128)
        kcp = S.sbuf("kcps", [64, 16 * CCH + 16], BF16)
        S.memset(kcp[:], 0.0, [kcp])
        hx = S.sbuf("hx", [128, CCH], F32)
        hsq = S.sbuf("hsq", [128, CCH], F32)
        htmp = S.sbuf("htmp", [128, CCH], F32)
        hg = S.sbuf("hg", [128, CCH], BF16)
        hbias = S.sbuf("hbias", [128, 1], F32)
        for j in range(2):
            for l in range(32):
                S.mm(PB[1][:, 0:1], w1b[:, j, l, :], peTb[:, j, l:l + 1], l == 0, l == 31, [w1b, peTb], [PB[1]])
            S.copy(hbias[:], PB[1][:, 0:1], [PB[1]], [hbias])
            for c0 in range(0, NCMP, CCH):
                n = min(CCH, NCMP - c0)
                ntok = 16 * (n - 1) + 32
                S.dma(kcp[:, 0:ntok].bitcast(F32), kcp_d.t[j, :, 8 * c0:8 * c0 + ntok // 2], reads=[kcp_d], writes=[kcp])
                for l in range(32):
                    S.mm(PB[0][:, 0:n], w1b[:, j, l, :], kcp[:, l:l + 16 * (n - 1) + 1:16], l == 0, l == 31,
                         [w1b, kcp], [PB[0]])
                if n < CCH:
                    S.memset(hg[:], 0.0, [hg])
                S.act(hx[:, 0:n], PB[0][:, 0:n], AF.Identity, [PB[0], hbias], [hx], bias=hbias[:, 0:1])
                gelu_tanh(S, hg[:, 0:n], hg, hx, hsq, htmp, n)
                if j == 0:
                    S.mm(PB[2][0:64, 0:n], w2b[:, 0, :], hg[:, 0:n], True, True, [w2b, hg], [PB[2]])
                    S.copy(kcT[:, c0:c0 + n], PB[2][0:64, 0:n], [PB[2]], [kcT], eng="act")
                else:
                    for tt_ in range((n + 127) // 128):
                        S.mm(PB[2][:, 0:64], hg[:, tt_ * 128:(tt_ + 1) * 128], w2b[:, 1, :], True, True, [hg, w2b], [PB[2]])
                        S.copy(vcx[:, c0 // 128 + tt_, 256:320], PB[2][:, 0:64], [PB[2]], [vcx], eng="act")
        if NCMP % 128:
            pass

        kmst = S.sbuf("kmst", [64, 2, NB], F32)
        kmb = S.sbuf("kmb", [64, 2, NB], BF16)
        S.dma(kmst[:], km_d.t[:, :, :].rearrange("h d b -> d h b"), reads=[km_d], writes=[kmst])
        S.copy(kmb[:], kmst[:], [kmst], [kmb])

        qraw = S.sbuf("qraw", [64, 4, 512], BF16)
        qrot = S.sbuf("qrot", [64, 2, 512], BF16)
        mq = S.sbuf("mqs", [64, 2, 512], BF16)
        gts = S.sbuf("gts", [128, 4, 6], F32)
        Eb = [S.sbuf("Eb%d" % i, [128, 512], BF16) for i in range(4)]
        Pb = [S.sbuf("Pb%d" % i, [128, 512], BF16) for i in range(2)]
        imp = S.sbuf("imp", [128, 4, 256], F32)
        sc = S.sbuf("sc", [128, 256], F32)
        sc2 = S.sbuf("sc2", [128, 256], F32)
        selb = S.sbuf("selb", [128, 256], BF16)
        m8 = S.sbuf("m8", [128, 16], F32)
        selT = S.sbuf("selT", [128, 2, 512], BF16)
        mselT = S.sbuf("mselT", [64, 2, 512], BF16)
        msc = S.sbuf("msc", [128, NB], F32)
        mselb = S.sbuf("mselb", [128, NB], BF16)
        rr = S.sbuf("rr", [128, 8], F32)
        oacc = S.sbuf("oacc", [128, 4, 128], F32)
        macc = S.sbuf("macc", [128, 4, 128], F32)
        oaccb = S.sbuf("oaccb", [128, 4, 128], BF16)
        maccb = S.sbuf("maccb", [128, 4, 128], BF16)
        oTs = S.sbuf("oTs", [65, 512], F32)
        kbuf = [S.sbuf("kbuf%d" % i, [64, 512], BF16) for i in range(2)]
        vbuf = [S.sbuf("vbuf%d" % i, [128, 4, 66], BF16) for i in range(2)]
        kwbuf = [S.sbuf("kwbuf%d" % i, [64, 512], BF16) for i in range(2)]
        vwbuf = [S.sbuf("vwbuf%d" % i, [128, 4, 66], BF16) for i in range(2)]
        mkbuf = [S.sbuf("mkbuf%d" % i, [64, 2, 512], BF16) for i in range(2)]
        mvbuf = [S.sbuf("mvbuf%d" % i, [128, 4, 2, 66], BF16) for i in range(2)]
        for i in range(2):
            S.memset(vbuf[i][:], 1.0, [vbuf[i]])
            S.memset(vwbuf[i][:], 1.0, [vwbuf[i]])
            S.memset(mvbuf[i][:], 1.0, [mvbuf[i]])
        cnt = {"e": 0, "m": 0, "o": 0, "ld": 0}

        def attn_unit(qT_ap, qbuf, kT_ap, kbuf_, vx_ap, vbuf_, mT_lhs, mT_rhs, mbufs, out_ps, first, last,
                      sel1=None, sel2=None):
            i = cnt["e"] % 4
            cnt["e"] += 1
            sps = PB[i]
            if mT_lhs is not None:
                S.mm(sps[:], kT_ap, qT_ap, True, False, [kbuf_, qbuf], [sps])
                S.mm(sps[:], mT_lhs, mT_rhs, False, True, mbufs, [sps])
            else:
                S.mm(sps[:], kT_ap, qT_ap, True, True, [kbuf_, qbuf], [sps])
            S.act(Eb[i][:], sps[:], AF.Exp, [sps], [Eb[i]], scale=SCALE)
            src = Eb[i]
            for (base, cm, step) in (sel1, sel2):
                if base is None:
                    continue
                S.op("pool", lambda e, src=src, base=base, cm=cm, step=step: e.affine_select(
                    out=src[:], in_=src[:], pattern=[[step, 512]], compare_op=ALU.is_ge, fill=0.0,
                    base=base, channel_multiplier=cm), [src], [src])
            pending.append((out_ps, vx_ap, src, first, last, vbuf_))
            flush(PIPE)

        pending = []
        PIPE = 3

        def flush(keep):
            while len(pending) > keep:
                out_ps, vx_ap, src, first, last, vbuf_ = pending.pop(0)
                S.mm(out_ps[0:65, :], vx_ap, src[:], first, last, [vbuf_, src], [out_ps])

        NOSEL = (None, None, None)
        OWNJ = [(0, 0, 0), (1, 64, 3)]

        def finalize(out_ps, acc, col0, gate_col, first_branch):
            flush(0)
            S.copy(oTs[:], out_ps[0:65, :], [out_ps], [oTs], eng="act")
            fp = PB[6]
            for tq in range(4):
                S.tr(fp[:, tq * 65:(tq + 1) * 65], oTs[:, tq * 128:(tq + 1) * 128], identf[0:65, 0:65], [oTs, identf], [fp])
            fv = fp[:, 0:260].rearrange("p (t c) -> p t c", c=65)
            S.ts(rr[:, 0:4], fv[:, :, 64], 1e-30, None, ALU.max, None, [fp], [rr])
            S.op("dve", lambda e: e.reciprocal(out=rr[:, 4:8], in_=rr[:, 0:4]), [rr], [rr])
            if gate_col is not None:
                S.tt(rr[:, 4:8], rr[:, 4:8], gts[:, :, gate_col], ALU.mult, [rr, gts], [rr])
            for tq in range(4):
                dst = acc[:, tq, col0:col0 + 64]
                if first_branch:
                    S.ts(dst, fv[:, tq, 0:64], rr[:, 4 + tq:5 + tq], None, ALU.mult, None, [fp, rr], [acc])
                else:
                    S.stt(dst, fv[:, tq, 0:64], rr[:, 4 + tq:5 + tq], dst, ALU.mult, ALU.add, [fp, rr, acc], [acc])

        for qc in range(NQC):
            q0 = qc * 512
            S.dma(qraw[:, 0:2, :].bitcast(F32), qrawA.t[:, :, q0 // 2:q0 // 2 + 256].rearrange("h d t -> d h t"), reads=[qrawA], writes=[qraw])
            S.dma(qraw[:, 2:4, :].bitcast(F32), qrawB.t[:, :, q0 // 2:q0 // 2 + 256].rearrange("h d t -> d h t"), reads=[qrawB], writes=[qraw])
            S.dma(qrot[:].bitcast(F32), qrot_d.t[:, :, q0 // 2:q0 // 2 + 256].rearrange("h d t -> d h t"), reads=[qrot_d], writes=[qrot])
            S.dma(mq[:].bitcast(F32), mq_d.t[:, :, q0 // 2:q0 // 2 + 256].rearrange("h d t -> d h t"), reads=[mq_d], writes=[mq])
            S.dma(gts[:], gates_d.t[q0:q0 + 512, :].rearrange("(t p) c -> p t c", p=128), reads=[gates_d], writes=[gts])
            tmax = q0 + 511
            cmax = (tmax - 31) // 16 if tmax >= 31 else -1
            cmax = min(cmax, NCMP - 1)
            nct = cmax // 128 + 1 if cmax >= 0 else 0
            S.memset(imp[:], 0.0, [imp], eng="dve")
            for j in range(4):
                for ct in range(nct):
                    i = cnt["e"] % 2
                    cnt["e"] += 1
                    sps = PB[i]
                    S.mm(sps[:], kcT[:, ct * 128:(ct + 1) * 128], qraw[:, j, :], True, True, [kcT, qraw], [sps])
                    S.act(Eb[i][:], sps[:], AF.Exp, [sps], [Eb[i]], scale=SCALE)
                    base = q0 - 2048 * ct - 31
                    if base - 16 * 127 < 0:
                        S.op("pool", lambda e, src=Eb[i], base=base: e.affine_select(
                            out=src[:], in_=src[:], pattern=[[1, 512]], compare_op=ALU.is_ge, fill=0.0,
                            base=base, channel_multiplier=-16), [Eb[i]], [Eb[i]])
                    for tq in range(4):
                        S.mm(PB[2 + tq][:, 0:321], Eb[i][:, tq * 128:(tq + 1) * 128], vcx[:, ct, :],
                             ct == 0, ct == nct - 1, [Eb[i], vcx], [PB[2 + tq]])
                if nct == 0:
                    continue
                for tq in range(4):
                    ps = PB[2 + tq]
                    S.ts(rr[:, 0:1], ps[:, 320:321], 1e-30, None, ALU.max, None, [ps], [rr])
                    S.op("dve", lambda e: e.reciprocal(out=rr[:, 1:2], in_=rr[:, 0:1]), [rr], [rr])
                    S.stt(imp[:, tq, :], ps[:, 0:256], rr[:, 1:2], imp[:, tq, :], ALU.mult, ALU.add, [ps, rr, imp], [imp])
                    for (jj, col0, gcol) in OWNJ:
                        if jj == j:
                            S.tt(rr[:, 2:3], rr[:, 1:2], gts[:, tq, gcol:gcol + 1], ALU.mult, [rr, gts], [rr])
                            S.ts(oacc[:, tq, col0:col0 + 64], ps[:, 256:320], rr[:, 2:3], None, ALU.mult, None,
                                 [ps, rr], [oacc])
            if nct == 0:
                S.memset(oacc[:], 0.0, [oacc], eng="dve")
            for tq in range(4):
                T = qc * 4 + tq
                S.copy(sc[:], imp[:, tq, :], [imp], [sc])
                S.memset(sc[:, 0:1], 1e4, [sc], eng="dve")
                lo = max(2 * T - 1, 0)
                S.memset(sc[0:64, lo:2 * T + 1], 1e4, [sc], eng="dve")
                S.memset(sc[64:128, 2 * T:2 * T + 2], 1e4, [sc], eng="dve")
                if 2 * T + 1 < 256:
                    S.memset(sc[0:64, 2 * T + 1:256], NEG, [sc], eng="dve")
                if 2 * T + 2 < 256:
                    S.memset(sc[64:128, 2 * T + 2:256], NEG, [sc], eng="dve")
                S.op("dve", lambda e: e.max(out=m8[:, 0:8], in_=sc[:]), [sc], [m8])
                S.op("dve", lambda e: e.match_replace(out=sc2[:], in_to_replace=m8[:, 0:8], in_values=sc[:],
                                                      imm_value=-3.0e38), [sc, m8], [sc2])
                S.op("dve", lambda e: e.max(out=m8[:, 8:16], in_=sc2[:]), [sc2], [m8])
                S.ts(selb[:], sc[:], m8[:, 15:16], None, ALU.is_ge, None, [sc, m8], [selb])
                if 2 * T + 1 < 256:
                    S.memset(selb[0:64, 2 * T + 1:256], 0.0, [selb], eng="dve")
                if 2 * T + 2 < 256:
                    S.memset(selb[64:128, 2 * T + 2:256], 0.0, [selb], eng="dve")
                tp = PB[7]
                tpv = tp[:].bitcast(BF16)
                for bt in range(2):
                    S.tr(tpv[:, bt * 128:(bt + 1) * 128], selb[:, bt * 128:(bt + 1) * 128], identb[:], [selb, identb], [tp])
                S.act(selT[:, :, tq * 128:(tq + 1) * 128], tpv[:, 0:256].rearrange("p (b q) -> p b q", q=128),
                      AF.Identity, [tp], [selT], scale=MASKBIG, bias=-MASKBIG)
            for h in range(2):
                for tq in range(4):
                    cur = (q0 + tq * 128) // 256
                    gp = PB[7]
                    S.mm(gp[:, 0:NB], mq[:, h, tq * 128:(tq + 1) * 128], kmb[:, h, :], True, True, [mq, kmb], [gp])
                    S.copy(msc[:], gp[:, 0:NB], [gp], [msc])
                    S.memset(msc[:, cur:NB], NEG, [msc], eng="dve")
                    S.op("dve", lambda e: e.max(out=m8[:, 0:8], in_=msc[:]), [msc], [m8])
                    S.ts(mselb[:], msc[:], m8[:, 2:3], None, ALU.is_ge, None, [msc, m8], [mselb])
                    S.memset(mselb[:, cur:NB], 0.0, [mselb], eng="dve")
                    S.memset(mselb[:, cur:cur + 1], 1.0, [mselb], eng="dve")
                    tpv = gp[:].bitcast(BF16)
                    S.tr(tpv[0:NB, 512:640], mselb[:, :], identb[:], [mselb, identb], [gp])
                    S.act(mselT[0:NB, h, tq * 128:(tq + 1) * 128], tpv[0:NB, 512:640], AF.Identity, [gp], [mselT],
                          scale=MASKBIG, bias=-MASKBIG)
            outS = [PB[4], PB[5]]
            nkg = qc + 1
            mo_out = [None, None]
            for kg in range(nkg):
                li = cnt["ld"] % 2
                cnt["ld"] += 1
                k0 = kg * 512
                S.dma(kbuf[li][:].bitcast(F32), ks_d.t[:, k0 // 2:k0 // 2 + 256], reads=[ks_d], writes=[kbuf[li]])
                S.dma(vbuf[li][:, :, 0:64].bitcast(F32),
                      vs_d.t[k0:k0 + 512, :].rearrange("(t p) c -> p t c", p=128), reads=[vs_d], writes=[vbuf[li]])
                for h in range(2):
                    for t4 in range(4):
                        kt = kg * 4 + t4
                        diag = kt >= 4 * qc
                        attn_unit(qrot[:, h, :], qrot, kbuf[li][:, t4 * 128:(t4 + 1) * 128], kbuf[li],
                                  vbuf[li][:, t4, 0:65], vbuf[li],
                                  Ex[:, kt % 64, :], selT[:, kt // 64, :], [Ex, selT],
                                  outS[h], kt == 0, kt == 4 * qc + 3,
                                  sel1=(q0 - 128 * kt, -1, 1) if diag else NOSEL, sel2=NOSEL)
            for h in range(2):
                finalize(outS[h], oacc, h * 64, 3 * h + 1, False)
            for h in range(2):
                wout = PB[4 + h]
                kts = list(range(max(0, 4 * qc - 4), 4 * qc + 4))
                for kg in sorted(set(k // 4 for k in kts)):
                    li = cnt["ld"] % 2
                    cnt["ld"] += 1
                    k0 = kg * 512
                    S.dma(kwbuf[li][:].bitcast(F32), kw_d.t[:, k0 // 2:k0 // 2 + 256], reads=[kw_d], writes=[kwbuf[li]])
                    S.dma(vwbuf[li][:, :, 0:64].bitcast(F32),
                          vw_d.t[k0:k0 + 512, :].rearrange("(t p) c -> p t c", p=128), reads=[vw_d], writes=[vwbuf[li]])
                    for t4 in range(4):
                        kt = kg * 4 + t4
                        diag = kt >= 4 * qc
                        attn_unit(qrot[:, h, :], qrot, kwbuf[li][:, t4 * 128:(t4 + 1) * 128], kwbuf[li],
                                  vwbuf[li][:, t4, 0:65], vwbuf[li], None, None, None,
                                  wout, kt == kts[0], kt == kts[-1],
                                  sel1=(q0 - 128 * kt, -1, 1) if diag else NOSEL,
                                  sel2=(128 * kt - q0 + 511, 1, -1) if not diag else NOSEL)
                finalize(wout, oacc, h * 64, 3 * h + 2, False)
            for h in range(2):
                mout = PB[4 + h]
                for kg in range(nkg):
                    li = cnt["ld"] % 2
                    cnt["ld"] += 1
                    k0 = kg * 512
                    S.dma(mkbuf[li][:, 0, :].bitcast(F32), mk_d.t[h, :, k0 // 2:k0 // 2 + 256], reads=[mk_d], writes=[mkbuf[li]])
                    S.dma(mvbuf[li][:, :, 0, 0:64].bitcast(F32),
                          mv_d.t[k0:k0 + 512, 32 * h:32 * h + 32].rearrange("(t p) c -> p t c", p=128),
                          reads=[mv_d], writes=[mvbuf[li]])
                    for t4 in range(4):
                        kt = kg * 4 + t4
                        diag = kt >= 4 * qc
                        attn_unit(mq[:, h, :], mq, mkbuf[li][:, 0, t4 * 128:(t4 + 1) * 128], mkbuf[li],
                                  mvbuf[li][:, t4, 0, 0:65], mvbuf[li],
                                  Rm[0:NB, kt // 2, :], mselT[0:NB, h, :], [Rm, mselT],
                                  mout, kt == 0, kt == 4 * qc + 3,
                                  sel1=(q0 - 128 * kt, -1, 1) if diag else NOSEL, sel2=NOSEL)
                finalize(mout, macc, h * 64, None, True)
            S.copy(oaccb[:], oacc[:], [oacc], [oaccb])
            S.copy(maccb[:], macc[:], [macc], [maccb])
            S.dma(onsa_d.t[q0:q0 + 512, :].rearrange("(t p) c -> p t c", p=128), oaccb[:].bitcast(F32),
                  reads=[oaccb], writes=[onsa_d])
            S.dma(omoba_d.t[q0:q0 + 512, :].rearrange("(t p) c -> p t c", p=128), maccb[:].bitcast(F32),
                  reads=[maccb], writes=[omoba_d])
        S.emit_block(st)
        S.stack = S.outer
    return nc


def _cat(resA, b, key):
    return [np.asarray(resA[4 * b + p][key]) for p in range(4)]


def host_inputs_C(resA, inp, l, S):
    maps = []
    peT = _f32(np.transpose(inp["cmp_pe"][l], (0, 2, 1)))
    w1 = _f32(inp["cmp_w1"][l])
    w2 = _f32(inp["cmp_w2"][l])
    for b in range(2):
        oT = np.concatenate(_cat(resA, b, "oT"), axis=2)
        oQ = np.concatenate(_cat(resA, b, "oQ"), axis=2)
        oM = np.concatenate(_cat(resA, b, "oM"), axis=0)
        oG = np.concatenate(_cat(resA, b, "oG"), axis=0)
        oK = np.concatenate(_cat(resA, b, "oK"), axis=2)
        for g in range(4):
            kv = g // 2
            own = [2 * g, 2 * g + 1]
            oth = [h for h in range(4 * kv, 4 * kv + 4) if h not in own]
            maps.append({
                "qraw": _f32(oQ[own + oth]),
                "qrot": _f32(oT[own]),
                "kcp": _f32(np.stack([oT[8 + kv], oT[10 + kv]])),
                "ksT": _f32(oT[12 + kv]), "kwT": _f32(oT[14 + kv]),
                "vsw": _f32(np.concatenate([oM[:, 32 * kv:32 * kv + 32], oM[:, 64 + 32 * kv:64 + 32 * kv + 32]], axis=1)),
                "gates": _f32(oG[:, 6 * g:6 * g + 6]),
                "mq": _f32(oT[[16 + 2 * g, 17 + 2 * g]]), "mk": _f32(oT[[24 + 2 * g, 25 + 2 * g]]),
                "mv": _f32(oM[:, 128 + 64 * g:128 + 64 * g + 64]),
                "kmean": _f32(oK[own]),
                "peT": peT, "w1": w1, "w2": w2,
            })
    return maps


def build_stageD1(TL, S=None, io=None, pfx=""):
    alone = S is None
    nc = bass.Bass("TRN2", target_bir_lowering=False) if alone else S.nc
    NT = TL // 128
    with ExitStack() as st:
        if alone:
            S = Sched(nc, st)
        S.stack = st
        S.pfx = pfx
        _in = (lambda n, sh, dt: S.din(n, sh, dt)) if alone else (lambda n, sh, dt: io[n])
        _out = (lambda n, sh, dt: S.dout(n, sh, dt)) if alone else (lambda n, sh, dt: io[n])
        xs = _in("xs", [TL, D], F32)
        tokmaj = (not alone) and ("oN" in io)
        if tokmaj:
            oN_d, oMo_d = io["oN"], io["oMo"]
        else:
            onT_d = _in("onT", [512, TL // 2], F32)
            omT_d = _in("omT", [512, TL // 2], F32)
        cT_d = _in("cT", [128, 8], F32)
        wada = _in("wada", [D, 3072], F32)
        bada = _in("bada", [128, 3072], F32)
        gat = _in("gattn", [128, D], F32)
        wG_d = _in("wG", [D, 2048], F32)
        wun_d = _in("wupn", [512, D], F32)
        wum_d = _in("wupm", [512, D], F32)
        wo_d = _in("wout", [D, D], F32)
        xo = _out("xo", [TL, D], F32)

        identb = make_ident(S, BF16, "identb")
        PB = [S.psum("PB%d" % i, [128, 512], F32) for i in range(8)]
        stage = S.sbuf("wstage", [128, 8, 512], F32)
        wG = S.sbuf("wGb", [128, 8, 2048], BF16)
        wun = S.sbuf("wunb", [128, 4, D], BF16)
        wum = S.sbuf("wumb", [128, 4, D], BF16)
        wo = S.sbuf("wob", [128, 8, D], BF16)
        load_cast_weight(S, wG, 0, wG_d, 2048, stage)
        load_cast_weight(S, wo, 0, wo_d, D, stage)
        for (dst, src) in ((wun, wun_d), (wum, wum_d)):
            for c0 in (0, 512):
                S.dma(stage[:, 0:4, :], src.t[:, c0:c0 + 512].rearrange("(k p) n -> p k n", p=128), writes=[stage])
                S.copy(dst[:, :, c0:c0 + 512], stage[:, 0:4, :], [stage], [dst], eng="pool")
        mod = compute_mod(S, cT_d, wada, bada, 3072, "modD", stage, ps=PB[0])
        gt = S.sbuf("gat", [128, D], F32)
        S.dma(gt[:], gat.t[:, :], writes=[gt])
        A1 = S.sbuf("A1", [128, D], F32)
        S.stt(A1[:], mod[:, 1024:2048], 1.0, gt[:], ALU.add, ALU.mult, [mod, gt], [A1])
        B1 = mod[:, 0:1024]
        GA1 = mod[:, 2048:3072]

        xt = [S.sbuf("xt%d" % i, [128, D], F32) for i in range(2)]
        scr = S.sbuf("scr", [128, D], F32)
        small = S.sbuf("small", [128, 4], F32)
        hb = S.sbuf("hb", [128, D], BF16)
        hT = S.sbuf("hT", [128, 8, 128], BF16)
        onT = S.sbuf("onTs", [128, 4, 128], BF16)
        omT = S.sbuf("omTs", [128, 4, 128], BF16)
        otm = S.sbuf("otm", [128, 2, 512], BF16)
        sg = S.sbuf("sg", [128, 16, 128], F32)
        y1 = S.sbuf("y1", [128, 8, 128], F32)
        y2 = S.sbuf("y2", [128, 8, 128], F32)
        yT = S.sbuf("yT", [128, 8, 128], BF16)
        xo_t = S.sbuf("xo_t", [128, D], F32)
        pstr = PB[7]
        for it in range(NT):
            t0 = it * 128
            xb = xt[it % 2]
            S.dma(xb[:], xs.t[t0:t0 + 128, :], reads=[xs], writes=[xb])
            if tokmaj:
                S.dma(otm[:, 0, :].bitcast(F32), oN_d.t[t0:t0 + 128, :], reads=[oN_d], writes=[otm])
                S.dma(otm[:, 1, :].bitcast(F32), oMo_d.t[t0:t0 + 128, :], reads=[oMo_d], writes=[otm])
                for j_, dst_ in ((0, onT), (1, omT)):
                    pv2 = PB[6][:].bitcast(BF16)
                    for k in range(4):
                        S.tr(pv2[:, k * 128:(k + 1) * 128], otm[:, j_, k * 128:(k + 1) * 128], identb[:], [otm, identb], [PB[6]])
                    S.copy(dst_[:], pv2[:, 0:512].rearrange("p (k t) -> p k t", t=128), [PB[6]], [dst_], eng="act")
            else:
                S.dma(onT[:].bitcast(F32), onT_d.t[:, t0 // 2:t0 // 2 + 64].rearrange("(k p) t -> p k t", p=128), writes=[onT])
                S.dma(omT[:].bitcast(F32), omT_d.t[:, t0 // 2:t0 // 2 + 64].rearrange("(k p) t -> p k t", p=128), writes=[omT])
            rmsnorm_mod_tile(S, xb[:], xb, A1, B1, hb[:], hb, scr, small, Bbuf=mod)
            pv = pstr[:].bitcast(BF16)
            for k in range(8):
                S.tr(pv[:, k * 128:(k + 1) * 128], hb[:, k * 128:(k + 1) * 128], identb[:], [hb, identb], [pstr])
            S.copy(hT[:], pv[:].rearrange("p (k t) -> p k t", t=128), [pstr], [hT], eng="act")
            for c in range(16):
                pb = PB[c // 4]
                for k in range(8):
                    S.mm(pb[:, (c % 4) * 128:(c % 4 + 1) * 128], wG[:, k, c * 128:(c + 1) * 128], hT[:, k, :],
                         k == 0, k == 7, [wG, hT], [pb])
                if c % 4 == 3:
                    S.act(sg[:, c - 3:c + 1, :], pb[:].rearrange("p (c t) -> p c t", t=128), AF.Sigmoid, [pb], [sg])
            for c in range(16):
                pb = PB[4 + (c // 4) % 2] if False else PB[c // 4]
                w = wun if c < 8 else wum
                o = onT if c < 8 else omT
                cc = c % 8
                for kk in range(4):
                    S.mm(pb[:, (c % 4) * 128:(c % 4 + 1) * 128], w[:, kk, cc * 128:(cc + 1) * 128], o[:, kk, :],
                         kk == 0, kk == 3, [w, o], [pb])
                if c % 4 == 3:
                    dst = y1 if c < 8 else y2
                    c4 = (c % 8) - 3
                    S.tt(dst[:, c4:c4 + 4, :], pb[:].rearrange("p (c t) -> p c t", t=128), sg[:, c - 3:c + 1, :],
                         ALU.mult, [pb, sg], [dst])
            S.tt(yT[:], y1[:], y2[:], ALU.add, [y1, y2], [yT])
            for half in range(2):
                pb = PB[4 + half]
                for c in range(8):
                    S.mm(pb[:], yT[:, c, :], wo[:, c, half * 512:(half + 1) * 512], c == 0, c == 7, [yT, wo], [pb])
                S.tt(scr[:, half * 512:(half + 1) * 512], pb[:], GA1[:, half * 512:(half + 1) * 512], ALU.mult,
                     [pb, mod], [scr])
            S.tt(xo_t[:], scr[:], xb[:], ALU.add, [scr, xb], [xo_t])
            S.dma(xo.t[t0:t0 + 128, :], xo_t[:], reads=[xo_t], writes=[xo])
        S.emit_block(st)
        S.stack = S.outer
    return nc


def host_inputs_D1(resC, inp, l, x_cur, S):
    TL = S // 4
    w_in = inp["w_in"][l]
    wG = _f32(w_in[:, C_GN:C_GN + 2048])
    wada = _f32(inp["w_ada"][l][:, 0:3072])
    bada = _f32(np.broadcast_to(inp["b_ada"][l][None, 0:3072], (128, 3072)))
    gat = _f32(np.broadcast_to(inp["g_attn"][l][None, :], (128, D)))
    maps = []
    for b in range(2):
        on = np.concatenate([np.ascontiguousarray(np.asarray(resC[4 * b + g]["onsa"])).view(ml_dtypes.bfloat16)
                             for g in range(4)], axis=1)
        om = np.concatenate([np.ascontiguousarray(np.asarray(resC[4 * b + g]["omoba"])).view(ml_dtypes.bfloat16)
                             for g in range(4)], axis=1)
        for g in range(4):
            sl = slice(g * TL, (g + 1) * TL)
            onT = np.ascontiguousarray(on[sl].T).view(np.float32)
            omT = np.ascontiguousarray(om[sl].T).view(np.float32)
            maps.append({
                "xs": _f32(x_cur[b, sl]), "onT": onT, "omT": omT,
                "cT": _f32(inp["c"][b].reshape(8, 128).T),
                "wada": wada, "bada": bada, "gattn": gat, "wG": wG,
                "wupn": _f32(inp["w_up_nsa"][l]), "wupm": _f32(inp["w_up_moba"][l]), "wout": _f32(inp["w_out"][l]),
            })
    return maps


def build_stageD2(TL, last, S=None, io=None, pfx=""):
    alone = S is None
    nc = bass.Bass("TRN2", target_bir_lowering=False) if alone else S.nc
    NT = TL // 128
    NE = 16384
    with ExitStack() as st:
        if alone:
            S = Sched(nc, st)
        S.stack = st
        S.pfx = pfx
        _in = (lambda n, sh, dt: S.din(n, sh, dt)) if alone else (lambda n, sh, dt: io[n])
        _out = (lambda n, sh, dt: S.dout(n, sh, dt)) if alone else (lambda n, sh, dt: io[n])
        xs = _in("xs", [TL, D], F32)
        cT_d = _in("cT", [128, 8], F32)
        wada = _in("wada", [D, 3072], F32)
        bada = _in("bada", [128, 3072], F32)
        gff = _in("gffn", [128, D], F32)
        gfin = _in("gfinal", [128, D], F32)
        wq_d = _in("wq", [D, 2048], F32)
        kT_d = _in("kT", [2, 128, 128], F32)
        UT_d = _in("UT", [D, NE], F32)
        V_d = _in("V", [NE, D], F32)
        xo = _out("xo", [TL, D], F32)
        UTb = S.dscr("UTb" + pfx, [128, 8, NE], BF16)
        Vb = S.dscr("Vb" + pfx, [128, 128, D], BF16)

        identb = make_ident(S, BF16, "identb")
        PB = [S.psum("PB%d" % i, [128, 512], F32) for i in range(8)]
        utb = [S.sbuf("utb%d" % i, [128, 8, 1024], BF16) for i in range(2)]
        vb = [S.sbuf("vb%d" % i, [128, 8, 1024], BF16) for i in range(2)]
        gall = S.sbuf("gall", [128, 8, 8, 128], BF16)
        cst = [gall.view("cst%d" % i, gall[:, 4 * i:4 * i + 4, :, :].rearrange("p a b c -> p (a b c)").rearrange(
            "p (k n) -> p k n", n=512)) for i in range(2)]
        engs = ["pool", "dve", "act"]
        r = 0
        for e0 in range(0, NE, 512):
            stg = utb[r % 2]
            sv = stg[:].bitcast(F32)
            S.dma(sv, UT_d.t[:, e0:e0 + 512].rearrange("(k p) n -> p k n", p=128), writes=[stg])
            S.copy(cst[r % 2][:], sv, [stg], [cst[r % 2]], eng=engs[r % 3])
            S.dma(UTb.t[:, :, e0:e0 + 512], cst[r % 2][:], reads=[cst[r % 2]], writes=[UTb])
            r += 1
        for t0 in range(0, 128, 4):
            stg = vb[r % 2]
            sv = stg[:].bitcast(F32).rearrange("p k (a n) -> p (k a) n", a=1) if False else stg[:].bitcast(F32)
            sv4 = sv.rearrange("p (t h) n -> p t (h n)", h=2)
            S.dma(sv4, V_d.t[t0 * 128:(t0 + 4) * 128, :].rearrange("(t p) d -> p t d", p=128), writes=[stg])
            cv = cst[r % 2][:].rearrange("p (t h) n -> p t (h n)", h=2)
            S.copy(cv, sv4, [stg], [cst[r % 2]], eng=engs[r % 3])
            S.dma(Vb.t[:, t0:t0 + 4, :], cv, reads=[cst[r % 2]], writes=[Vb])
            r += 1
        wq = S.sbuf("wqb", [128, 8, 2048], BF16)
        stage = vb[0].view("wstage", vb[0][:].bitcast(F32))
        load_cast_weight(S, wq, 0, wq_d, 2048, stage)
        kst = S.sbuf("kst", [128, 2, 128], F32)
        kTb = S.sbuf("kTb", [128, 2, 128], BF16)
        S.dma(kst[:], kT_d.t[:, :, :].rearrange("j d n -> d j n"), writes=[kst])
        S.copy(kTb[:], kst[:], [kst], [kTb])
        mod = compute_mod(S, cT_d, wada, bada, 3072, "modE", stage, ps=PB[0])
        gel = S.sbuf("gel", [128, 3, 1024], F32)
        gt = gel.view("gff", gel[:, 0, :])
        S.dma(gt[:], gff.t[:, :], writes=[gt])
        A2 = S.sbuf("A2", [128, D], F32)
        S.stt(A2[:], mod[:, 1024:2048], 1.0, gt[:], ALU.add, ALU.mult, [mod, gt], [A2])
        B2 = mod[:, 0:1024]
        GA2 = mod[:, 2048:3072]
        gfn = S.sbuf("gfn", [128, D], F32)
        if last:
            S.dma(gfn[:], gfin.t[:, :], writes=[gfn])

        xt = [S.sbuf("xt%d" % i, [128, D], F32) for i in range(2)]
        scr = S.sbuf("scr", [128, D], F32)
        small = S.sbuf("small", [128, 4], F32)
        hb = S.sbuf("hb", [128, D], BF16)
        hT = S.sbuf("hT", [128, 8, 128], BF16)
        qT = gall.view("qT", gall[:, 0:2, :, :].rearrange("p a b c -> p (a b) c"))
        sc = S.sbuf("sc", [128, 16, 128], F32)
        m16 = S.sbuf("m16", [128, 16, 16], F32)
        tmpa = S.sbuf("tmpa", [128, 256], F32)
        c16 = S.sbuf("c16", [128, 8, 16], F32)
        e16 = S.sbuf("e16", [128, 16], F32)
        st8 = S.sbuf("st8", [128, 4, 8], F32)
        xg = [S.sbuf("xg%d" % i, [128, 8, 128], F32) for i in range(2)]
        exb = [S.sbuf("exb%d" % i, [128, 8, 128], BF16) for i in range(2)]
        gab = [S.sbuf("gab%d" % i, [128, 8, 128], BF16) for i in range(2)]
        cnt = {"x": 0, "ld": 0}
        hTs = [hT, S.sbuf("hT1", [128, 8, 128], BF16)]
        scs = [sc, S.sbuf("sc1", [128, 16, 128], F32)]
        c16s = [c16, S.sbuf("c161", [128, 8, 16], F32)]
        st8s = [st8, S.sbuf("st81", [128, 4, 8], F32)]
        assert NT % 2 == 0
        for ip in range(NT // 2):
            for u in range(2):
                it = 2 * ip + u
                t0 = it * 128
                xb = xt[u]
                hT_, sc_, c16_, st8_ = hTs[u], scs[u], c16s[u], st8s[u]
                S.dma(xb[:], xs.t[t0:t0 + 128, :], reads=[xs], writes=[xb])
                rmsnorm_mod_tile(S, xb[:], xb, A2, B2, hb[:], hb, scr, small, Bbuf=mod)
                pstr = PB[4]
                pv = pstr[:].bitcast(BF16)
                for k in range(8):
                    S.tr(pv[:, k * 128:(k + 1) * 128], hb[:, k * 128:(k + 1) * 128], identb[:], [hb, identb], [pstr])
                S.copy(hT_[:], pv[:].rearrange("p (k t) -> p k t", t=128), [pstr], [hT_], eng="act")
                for c in range(16):
                    pb = PB[c // 4]
                    for k in range(8):
                        S.mm(pb[:, (c % 4) * 128:(c % 4 + 1) * 128], wq[:, k, c * 128:(c + 1) * 128], hT_[:, k, :],
                             k == 0, k == 7, [wq, hT_], [pb])
                    if c % 4 == 3:
                        S.copy(qT[:, c - 3:c + 1, :], pb[:].rearrange("p (c t) -> p c t", t=128), [pb], [qT], eng="act")
                for c in range(16):
                    pb = PB[c // 4]
                    S.mm(pb[:, (c % 4) * 128:(c % 4 + 1) * 128], qT[:, c, :], kTb[:, c % 2, :], True, True, [qT, kTb], [pb])
                    if c % 4 == 3:
                        S.copy(sc_[:, c - 3:c + 1, :], pb[:].rearrange("p (c t) -> p c t", t=128), [pb], [sc_], eng="act")
                for c in range(16):
                    S.op("dve", lambda e, c=c, sc_=sc_: e.max(out=m16[:, c, 0:8], in_=sc_[:, c, :]), [sc_], [m16])
                    S.op("dve", lambda e, c=c, sc_=sc_: e.match_replace(out=tmpa[:, 0:128], in_to_replace=m16[:, c, 0:8],
                                                                        in_values=sc_[:, c, :], imm_value=-3.0e38), [sc_, m16], [tmpa])
                    S.op("dve", lambda e, c=c: e.max(out=m16[:, c, 8:16], in_=tmpa[:, 0:128]), [tmpa], [m16])
                cand = gel[:, 0:2, :].rearrange("p a (h x) -> p (a h) x", x=256)
                m4 = m16[:].rearrange("p (h two) k -> p h two k", two=2)
                S.tt(cand.rearrange("p h (a b) -> p h a b", b=16),
                     m4[:, :, 0, :].unsqueeze(3).to_broadcast([128, 8, 16, 16]),
                     m4[:, :, 1, :].unsqueeze(2).to_broadcast([128, 8, 16, 16]), ALU.add, [m16], [gel])
                for h in range(8):
                    S.op("dve", lambda e, h=h, c16_=c16_: e.max(out=c16_[:, h, 0:8], in_=cand[:, h, :]), [gel], [c16_])
                    S.op("dve", lambda e, h=h, c16_=c16_: e.match_replace(out=tmpa[:], in_to_replace=c16_[:, h, 0:8],
                                                                          in_values=cand[:, h, :], imm_value=-3.0e38), [gel, c16_], [tmpa])
                    S.op("dve", lambda e, h=h, c16_=c16_: e.max(out=c16_[:, h, 8:16], in_=tmpa[:]), [tmpa], [c16_])
                S.ts(st8_[:, 0, :], c16_[:, :, 0], -1.0, None, ALU.mult, None, [c16_], [st8_])
                for h in range(8):
                    S.act(e16[:], c16_[:, h, :], AF.Exp, [c16_, st8_], [e16, st8_], bias=st8_[:, 0, h:h + 1],
                          accum_out=st8_[:, 1, h:h + 1])
                S.act(st8_[:, 2, :], st8_[:, 1, :], AF.Ln, [st8_], [st8_])
                S.tt(st8_[:, 3, :], st8_[:, 0, :], st8_[:, 2, :], ALU.subtract, [st8_], [st8_])
            for ec in range(16):
                li = cnt["ld"] % 2
                cnt["ld"] += 1
                S.dma(utb[li][:], UTb.t[:, :, ec * 1024:(ec + 1) * 1024], reads=[UTb], writes=[utb[li]])
                S.dma(vb[li][:], Vb.t[:, ec * 8:(ec + 1) * 8, :], reads=[Vb], writes=[vb[li]])
                for u in range(2):
                    hT_, sc_, c16_, st8_ = hTs[u], scs[u], c16s[u], st8s[u]
                    for h in range(8):
                        xi = cnt["x"] % 2
                        cnt["x"] += 1
                        S.tt(xg[xi][:], sc_[:, 2 * h, ec * 8:(ec + 1) * 8].unsqueeze(2).to_broadcast([128, 8, 128]),
                             sc_[:, 2 * h + 1, :].unsqueeze(1).to_broadcast([128, 8, 128]), ALU.add, [sc_], [xg[xi]])
                        S.act(exb[xi][:], xg[xi][:], AF.Exp, [xg[xi], st8_], [exb[xi]], bias=st8_[:, 3, h:h + 1])
                        S.stt(gall[:, h, :, :], xg[xi][:], c16_[:, h, 15:16], exb[xi][:], ALU.is_ge, ALU.mult,
                              [xg[xi], c16_, exb[xi]], [gall])
                    for ii in range(8):
                        pb = PB[2 + ii // 4]
                        for k in range(8):
                            S.mm(pb[:, (ii % 4) * 128:(ii % 4 + 1) * 128], utb[li][:, k, ii * 128:(ii + 1) * 128], hT_[:, k, :],
                                 k == 0, k == 7, [utb[li], hT_], [pb])
                    for ii in range(8):
                        pb = PB[ii // 4]
                        for h in range(8):
                            S.mm(pb[:, (ii % 4) * 128:(ii % 4 + 1) * 128], gall[:, h, ii, :], identb[:], h == 0, h == 7,
                                 [gall, identb], [pb])
                    S.copy(gel[:, 0, 0:512], PB[2][:], [PB[2]], [gel], eng="act")
                    S.copy(gel[:, 0, 512:1024], PB[3][:], [PB[3]], [gel], eng="act")
                    S.act(gel[:, 1, :], gel[:, 0, :], AF.Square, [gel], [gel])
                    S.ts(gel[:, 1, :], gel[:, 1, :], 0.044715, 1.0, ALU.mult, ALU.add, [gel], [gel])
                    S.tt(gel[:, 2, :], gel[:, 1, :], gel[:, 0, :], ALU.mult, [gel], [gel])
                    S.act(gel[:, 1, :], gel[:, 2, :], AF.Sigmoid, [gel], [gel], scale=GELU_C)
                    S.tt(gel[:, 2, :], gel[:, 0, :], gel[:, 1, :], ALU.mult, [gel], [gel])
                    gb = gab[u]
                    for hf in range(2):
                        S.tt(gb[:, hf * 4:(hf + 1) * 4, :], gel[:, 2, hf * 512:(hf + 1) * 512].rearrange("p (c t) -> p c t", t=128),
                             PB[hf][:].rearrange("p (c t) -> p c t", t=128), ALU.mult, [gel, PB[hf]], [gb])
                    for ii in range(8):
                        for hf in range(2):
                            S.mm(PB[4 + 2 * u + hf][:], gb[:, ii, :], vb[li][:, ii, hf * 512:(hf + 1) * 512],
                                 ec == 0 and ii == 0, ec == 15 and ii == 7, [gb, vb[li]], [PB[4 + 2 * u + hf]])
            for u in range(2):
                it = 2 * ip + u
                t0 = it * 128
                xb = xt[u]
                for hf in range(2):
                    S.tt(scr[:, hf * 512:(hf + 1) * 512], PB[4 + 2 * u + hf][:], GA2[:, hf * 512:(hf + 1) * 512], ALU.mult,
                         [PB[4 + 2 * u + hf], mod], [scr])
                xo_t = xb
                S.tt(xo_t[:], scr[:], xb[:], ALU.add, [scr, xb], [xo_t])
                if last:
                    fo = gel.view("fo", gel[:, 0, :])
                    fs = gel.view("fs", gel[:, 1, :])
                    rmsnorm_mod_tile(S, xo_t[:], xo_t, gfn, None, fo[:], fo, fs, small)
                    S.dma(xo.t[t0:t0 + 128, :], fo[:], reads=[fo], writes=[xo])
                else:
                    S.dma(xo.t[t0:t0 + 128, :], xo_t[:], reads=[xo_t], writes=[xo])
        S.emit_block(st)
        S.stack = S.outer
    return nc


def host_inputs_D2(x1_list, inp, l, S):
    wada = _f32(inp["w_ada"][l][:, 3072:6144])
    bada = _f32(np.broadcast_to(inp["b_ada"][l][None, 3072:6144], (128, 3072)))
    gff = _f32(np.broadcast_to(inp["g_ffn"][l][None, :], (128, D)))
    gfin = _f32(np.broadcast_to(inp["g_final"][None, :], (128, D)))
    kT = _f32(np.stack([inp["peer_k1"][l].T, inp["peer_k2"][l].T]))
    UT = _f32(inp["peer_u"][l].T)
    V = _f32(inp["peer_v"][l])
    wq = _f32(inp["peer_wq"][l])
    maps = []
    for cid in range(8):
        b = cid // 4
        maps.append({"xs": _f32(x1_list[cid]), "cT": _f32(inp["c"][b].reshape(8, 128).T), "wada": wada, "bada": bada,
                     "gffn": gff, "gfinal": gfin, "wq": wq, "kT": kT, "UT": UT, "V": V})
    return maps


def build_fused(SQ):
    nc = bass.Bass("TRN2", target_bir_lowering=False)
    NB = SQ // 256
    with ExitStack() as outer:
        S = Sched(nc, outer)
        x_in = S.din("x", [SQ, D], F32)
        cT = S.din("cT", [128, 8], F32)
        pos = S.din("pos", [16, SQ], F32)
        gfin = S.din("gfinal", [128, D], F32)
        out = S.dout("out", [SQ, D], F32)
        xcur = x_in
        for l in range(2):
            P = "L%d_" % l
            wada = S.din(P + "wada", [D, 6144], F32)
            bada = S.din(P + "bada", [128, 6144], F32)
            gat = S.din(P + "gattn", [128, D], F32)
            gff = S.din(P + "gffn", [128, D], F32)
            ext = {n: S.din(P + n, sh, F32) for n, sh in (
                ("wT", [D, NHT * 64]), ("wS", [D, 28 * 64]), ("wM", [D, NM]),
                ("peT", [2, 64, 32]), ("w1", [2, 2048, 128]), ("w2", [2, 128, 64]),
                ("wG", [D, 2048]), ("wupn", [512, D]), ("wupm", [512, D]), ("wout", [D, D]),
                ("wq", [D, 2048]), ("kT", [2, 128, 128]), ("UT", [D, 16384]), ("V", [16384, D]))}
            scr = {n: S.dscr(P + n, sh, F32) for n, sh in (
                ("oT", [NHT, 64, SQ // 2]), ("oQ", [8, 64, SQ // 2]), ("oM", [SQ, 384]), ("oG", [SQ, 24]),
                ("oK", [8, 64, NB]), ("oN", [SQ, 256]), ("oMo", [SQ, 256]), ("x1", [SQ, D]), ("x2", [SQ, D]))}
            ioA = dict(xs=xcur, cT=cT, wada=wada.view("wadaA", wada.t[:, 0:2048]), bada=bada.view("badaA", bada.t[:, 0:2048]),
                       gattn=gat, wT=ext["wT"], wS=ext["wS"], wM=ext["wM"], pos=pos,
                       oT=scr["oT"], oQ=scr["oQ"], oM=scr["oM"], oG=scr["oG"], oK=scr["oK"])
            build_stageA(SQ, S=S, io=ioA, pfx=P + "A_")
            for g in range(4):
                ioC = dict(oT=scr["oT"], oQ=scr["oQ"], oM=scr["oM"], oG=scr["oG"], oK=scr["oK"],
                           oN=scr["oN"], oMo=scr["oMo"], peT=ext["peT"], w1=ext["w1"], w2=ext["w2"])
                build_stageC(SQ, S=S, io=ioC, pfx=P + "C%d_" % g, g=g)
            ioD1 = dict(xs=xcur, oN=scr["oN"], oMo=scr["oMo"], cT=cT,
                        wada=wada.view("wadaD1", wada.t[:, 0:3072]), bada=bada.view("badaD1", bada.t[:, 0:3072]),
                        gattn=gat, wG=ext["wG"], wupn=ext["wupn"], wupm=ext["wupm"], wout=ext["wout"], xo=scr["x1"])
            build_stageD1(SQ, S=S, io=ioD1, pfx=P + "D1_")
            last = (l == 1)
            ioD2 = dict(xs=scr["x1"], cT=cT, wada=wada.view("wadaD2", wada.t[:, 3072:6144]),
                        bada=bada.view("badaD2", bada.t[:, 3072:6144]), gffn=gff, gfinal=gfin,
                        wq=ext["wq"], kT=ext["kT"], UT=ext["UT"], V=ext["V"], xo=(out if last else scr["x2"]))
            build_stageD2(SQ, last, S=S, io=ioD2, pfx=P + "D2_")
            xcur = scr["x2"]
    return nc


def host_inputs_fused(inp, S):
    maps = []
    per_layer = []
    for l in range(2):
        w_in = inp["w_in"][l]
        wT = np.concatenate([w_in[:, c:c + 64] for c in HT_COLS], axis=1)
        sw = []
        for i in ROPE_IDX:
            c = HT_COLS[i]
            sw += [w_in[:, c + 8:c + 16], w_in[:, c:c + 8], np.zeros((D, 48), np.float32)]
        P = "L%d_" % l
        per_layer.append({
            P + "wada": _f32(inp["w_ada"][l]), P + "bada": _f32(np.broadcast_to(inp["b_ada"][l][None, :], (128, 6144))),
            P + "gattn": _f32(np.broadcast_to(inp["g_attn"][l][None, :], (128, D))),
            P + "gffn": _f32(np.broadcast_to(inp["g_ffn"][l][None, :], (128, D))),
            P + "wT": _f32(wT), P + "wS": _f32(np.concatenate(sw, axis=1)),
            P + "wM": _f32(np.concatenate([w_in[:, C_VS:C_VS + 128], w_in[:, C_VW:C_VW + 128], w_in[:, C_MV:C_MV + 512],
                                           w_in[:, C_NG:C_NG + 24]], axis=1)),
            P + "peT": _f32(np.transpose(inp["cmp_pe"][l], (0, 2, 1))), P + "w1": _f32(inp["cmp_w1"][l]),
            P + "w2": _f32(inp["cmp_w2"][l]), P + "wG": _f32(w_in[:, C_GN:C_GN + 2048]),
            P + "wupn": _f32(inp["w_up_nsa"][l]), P + "wupm": _f32(inp["w_up_moba"][l]), P + "wout": _f32(inp["w_out"][l]),
            P + "wq": _f32(inp["peer_wq"][l]), P + "kT": _f32(np.stack([inp["peer_k1"][l].T, inp["peer_k2"][l].T])),
            P + "UT": _f32(inp["peer_u"][l].T), P + "V": _f32(inp["peer_v"][l]),
        })
    pos = _f32(np.broadcast_to(np.arange(S, dtype=np.float32)[None, :], (16, S)))
    gfin = _f32(np.broadcast_to(inp["g_final"][None, :], (128, D)))
    for cid in range(8):
        b = cid // 4
        m = {"x": _f32(inp["x"][b]), "cT": _f32(inp["c"][b].reshape(8, 128).T), "pos": pos, "gfinal": gfin}
        m.update(per_layer[0]); m.update(per_layer[1])
        maps.append(m)
    return maps


_PROGS = {}


def _prog(key, builder):
    if key not in _PROGS:
        _PROGS[key] = builder()
    return _PROGS[key]


def _run(nc, maps):
    res = run_bass_kernel_spmd(nc, maps, core_ids=list(range(8)))
    return [{k: np.asarray(v) for k, v in r.items()} for r in res.results]


FUSED = False


def kernel(**inputs):
    inp = {k: np.asarray(v) for k, v in inputs.items()}
    B, S, _ = inp["x"].shape
    assert B == 2
    if FUSED:
        res = _run(_prog(("F", S), lambda: build_fused(S)), host_inputs_fused(inp, S))
        return np.ascontiguousarray(np.stack([res[0]["out"], res[4]["out"]]), dtype=np.float32)
    TL = S // 4
    x = np.asarray(inp["x"], dtype=np.float32)
    for l in range(2):
        resA = _run(_prog(("A", TL), lambda: build_stageA(TL)), host_inputs_A(inp, l, x, S))
        resC = _run(_prog(("C", S), lambda: build_stageC(S)), host_inputs_C(resA, inp, l, S))
        del resA
        resD1 = _run(_prog(("D1", TL), lambda: build_stageD1(TL)), host_inputs_D1(resC, inp, l, x, S))
        del resC
        x1 = [r["xo"] for r in resD1]
        last = (l == 1)
        resD2 = _run(_prog(("D2", TL, last), lambda: build_stageD2(TL, last)), host_inputs_D2(x1, inp, l, S))
        x = np.stack([np.concatenate([resD2[4 * b + g]["xo"] for g in range(4)], axis=0) for b in range(2)])
    return np.ascontiguousarray(x, dtype=np.float32)
```
